# Optimizing a Trainium2 kernel written in Bass

```python
import math
import jax, jax.numpy as jnp
from jax import lax
import numpy as np

D_MODEL = 1024
BATCH = 1
SEQ = 16384
DEPTH = 4
DEC_BATCH = 32
DEC_SEQ = 2048
PAST_LEN = 128

N_MIXERS = 3
N_ATTN_LAYERS = (DEPTH + 2) // 3
N_CONV_LAYERS = (DEPTH + 1) // 3
N_HYENA_LAYERS = DEPTH // 3

N_HEADS = 8
HEAD_DIM = 64
Q_BLOCK = 128

CONV_WIDTH = 31

HYENA_ORDER = 2
SHORT_WIDTH = 3
POS_BANDS = 16
POS_EMB_DIM = 1 + 2 * POS_BANDS
FILTER_WIDTH = 64
FAST_DECAY_PCT = 0.3
SLOW_DECAY_PCT = 1.5
DECAY_TARGET = 1e-2
MAX_DECAY = math.log(DECAY_TARGET) / FAST_DECAY_PCT
MIN_DECAY = math.log(DECAY_TARGET) / SLOW_DECAY_PCT

N_GROUPS = 4
EXPERTS_PER_GROUP = 4
N_EXPERTS = N_GROUPS * EXPERTS_PER_GROUP
TOP_K = 2
EXPERT_FF = 512

PLE_DIM = 256

ALPHA = (2 * DEPTH) ** 0.25
BETA = (8 * DEPTH) ** -0.25
LN_EPS = 1e-5

kernel_name = 'hybrid_diffattn_conformer_hyena_hmoe_encoder'


def layer_norm(x, g, b):
    xf = x.astype(jnp.float32)
    mu = jnp.mean(xf, axis=-1, keepdims=True)
    var = jnp.mean(jnp.square(xf - mu), axis=-1, keepdims=True)
    return ((xf - mu) * lax.rsqrt(var + LN_EPS) * g + b).astype(x.dtype)


def rms_norm(x, g):
    xf = x.astype(jnp.float32)
    ms = jnp.mean(jnp.square(xf), axis=-1, keepdims=True)
    return (xf * lax.rsqrt(ms + LN_EPS) * g).astype(x.dtype)


def depthwise_conv(x, w, b):
    width = w.shape[0]
    pad = width // 2
    y = lax.conv_general_dilated(x, w[:, None, :].astype(x.dtype), (1,), [(pad, pad)],
                                 dimension_numbers=('NWC', 'WIO', 'NWC'),
                                 feature_group_count=x.shape[-1])
    return y + b


def alibi_slopes(n_heads):
    return 2.0 ** (-8.0 * (jnp.arange(n_heads, dtype=jnp.float32) + 1.0) / n_heads)


def diff_attention(x, w_qkv, w_o, lam_q1, lam_k1, lam_q2, lam_k2, subln_g, lambda_init):
    B, L, _ = x.shape
    f32 = jnp.float32
    n_blk = L // Q_BLOCK
    q, k, v = jnp.split(x @ w_qkv, 3, axis=-1)
    q = q.reshape(B, L, N_HEADS, 2, HEAD_DIM) * (HEAD_DIM ** -0.5)
    k = k.reshape(B, L, N_HEADS, 2, HEAD_DIM)
    v = v.reshape(B, L, N_HEADS, 2 * HEAD_DIM)
    lam = (jnp.exp(jnp.sum(lam_q1.astype(f32) * lam_k1.astype(f32)))
           - jnp.exp(jnp.sum(lam_q2.astype(f32) * lam_k2.astype(f32))) + lambda_init)
    slopes = alibi_slopes(N_HEADS)[:, None, None, None]
    k_pos = jnp.arange(L, dtype=f32)
    q_blocks = jnp.moveaxis(q.reshape(B, n_blk, Q_BLOCK, N_HEADS, 2, HEAD_DIM), 1, 0)
    starts = jnp.arange(n_blk, dtype=f32) * Q_BLOCK

    def attend(args):
        q_blk, start = args
        s = jnp.einsum('bqhcd,bkhcd->bhcqk', q_blk, k, preferred_element_type=f32)
        q_pos = start + jnp.arange(Q_BLOCK, dtype=f32)
        dist = jnp.abs(q_pos[:, None] - k_pos[None, :])
        p = jax.nn.softmax(s - slopes * dist, axis=-1)
        a = p[:, :, 0] - lam * p[:, :, 1]
        return jnp.einsum('bhqk,bkhe->bqhe', a.astype(v.dtype), v)

    o = lax.map(attend, (q_blocks, starts))
    o = jnp.moveaxis(o, 0, 1).reshape(B, L, N_HEADS, 2 * HEAD_DIM)
    o = rms_norm(o, subln_g) * (1.0 - lambda_init)
    return o.reshape(B, L, D_MODEL) @ w_o


def conformer_conv(x, w_pw1, b_pw1, w_dw, b_dw, ln_g, ln_b, w_pw2, b_pw2):
    a, g = jnp.split(x @ w_pw1 + b_pw1, 2, axis=-1)
    h = a * jax.nn.sigmoid(g)
    h = depthwise_conv(h, w_dw, b_dw)
    h = jax.nn.silu(layer_norm(h, ln_g, ln_b))
    return h @ w_pw2 + b_pw2


def hyena_filters(L, f_w1, f_b1, f_w2, f_b2, f_w3, f_b3, f_freq, f_wout):
    f32 = jnp.float32
    t = jnp.linspace(0.0, 1.0, L, dtype=f32)[:, None]
    w = 2.0 * math.pi * jnp.arange(L, dtype=f32)[:, None] / L
    bands = jnp.linspace(1e-4, POS_BANDS - 1, POS_BANDS, dtype=f32)[None, :]
    fw = bands * w
    z = jnp.concatenate([t, jnp.cos(fw), -jnp.sin(fw)], axis=-1)
    h = jnp.sin(f_freq * (z @ f_w1 + f_b1))
    h = jnp.sin(f_freq * (h @ f_w2 + f_b2))
    h = jnp.sin(f_freq * (h @ f_w3 + f_b3))
    h = (h @ f_wout).astype(f32).reshape(L, HYENA_ORDER, 2, D_MODEL)
    deltas = jnp.abs(jnp.linspace(MIN_DECAY, MAX_DECAY, D_MODEL, dtype=f32))
    decay = jnp.exp(-t * deltas)
    return h * decay[:, None, None, :]


def bidir_long_conv(v, h_fwd, h_bwd, bias):
    L = v.shape[1]
    n_fft = 2 * L
    h_two_sided = jnp.concatenate([h_fwd[:1] + h_bwd[:1], h_fwd[1:],
                                   jnp.zeros_like(h_fwd[:1]), h_bwd[:0:-1]], axis=0)
    vf = v.astype(jnp.float32)
    spec = jnp.fft.rfft(vf, n=n_fft, axis=1) * jnp.fft.rfft(h_two_sided, n=n_fft, axis=0)[None]
    y = jnp.fft.irfft(spec, n=n_fft, axis=1)[:, :L]
    return (y + vf * bias.astype(jnp.float32)).astype(v.dtype)


def hyena_operator(x, w_in, b_in, w_short, b_short, f_w1, f_b1, f_w2, f_b2, f_w3, f_b3,
                   f_freq, f_wout, f_bias, w_out, b_out):
    L = x.shape[1]
    u = depthwise_conv(x @ w_in + b_in, w_short, b_short)
    x1, x2, v = jnp.split(u, 3, axis=-1)
    h = hyena_filters(L, f_w1, f_b1, f_w2, f_b2, f_w3, f_b3, f_freq, f_wout)
    z = v
    for n, gate in enumerate((x1, x2)):
        z = gate * bidir_long_conv(z, h[:, n, 0], h[:, n, 1], f_bias[n])
    return z @ w_out + b_out


def hier_moe(x, w_group, b_group, w_expert, b_expert, w_gate, w_up, w_down):
    B, L, D = x.shape
    xt = x.reshape(-1, D)
    T = xt.shape[0]
    g_logits = (xt @ w_group + b_group).astype(jnp.float32)
    g_idx = jnp.argmax(g_logits, axis=-1)
    g_w = jnp.max(jax.nn.softmax(g_logits, axis=-1), axis=-1, keepdims=True)
    e_logits = (xt @ w_expert + b_expert).astype(jnp.float32).reshape(T, N_GROUPS, EXPERTS_PER_GROUP)
    e_logits = e_logits[jnp.arange(T), g_idx]
    top_v, top_i = lax.top_k(e_logits, TOP_K)
    top_w = jax.nn.softmax(top_v, axis=-1) * g_w
    flat = g_idx[:, None] * EXPERTS_PER_GROUP + top_i
    comb = jnp.sum(jax.nn.one_hot(flat, N_EXPERTS, dtype=jnp.float32) * top_w[..., None], axis=1)
    comb = comb.astype(x.dtype)
    y = jnp.zeros_like(xt)
    for e in range(N_EXPERTS):
        hid = jax.nn.silu(xt @ w_gate[e]) * (xt @ w_up[e])
        y = y + (hid @ w_down[e]) * comb[:, e:e + 1]
    return y.reshape(B, L, D)


def trunk(x, p, attn, conv, hyena, norms, moe, ple):
    for i in range(DEPTH):
        j = i // N_MIXERS
        kind = i % N_MIXERS
        if kind == 0:
            h = diff_attention(x, *[a[j] for a in attn], lambda_init=0.8 - 0.6 * math.exp(-0.3 * i))
        elif kind == 1:
            h = conformer_conv(x, *[a[j] for a in conv])
        else:
            h = hyena_operator(x, *[a[j] for a in hyena])
        ln1_g, ln1_b, ln2_g, ln2_b = [a[i] for a in norms]
        x = layer_norm(ALPHA * x + h, ln1_g, ln1_b)
        x = layer_norm(ALPHA * x + hier_moe(x, *[a[i] for a in moe]), ln2_g, ln2_b)
        ple_up, ple_gate = ple[0][i], ple[1][i]
        x = x + (p[i] @ ple_up) * jax.nn.sigmoid(x @ ple_gate)
    return x


def setup_inputs(seed: int = 0) -> dict:
    key = jax.random.key(seed)
    keys = iter(jax.random.split(key, 64))
    D = D_MODEL
    NA, NC, NH = N_ATTN_LAYERS, N_CONV_LAYERS, N_HYENA_LAYERS

    def normal(shape, scale):
        return jax.random.normal(next(keys), shape, jnp.float32) * scale

    def gain(shape):
        return 1.0 + normal(shape, 0.02)

    return {
        'x_prompt': normal((BATCH, SEQ, D), 1.0),
        'x_sample': normal((DEC_BATCH, DEC_SEQ, D), 1.0),
        'p_prompt': normal((DEPTH, BATCH, SEQ, PLE_DIM), 1.0),
        'p_sample': normal((DEPTH, DEC_BATCH, DEC_SEQ, PLE_DIM), 1.0),
        'attn_w_qkv': jnp.concatenate([normal((NA, D, 2 * D), D ** -0.5),
                                       normal((NA, D, D), D ** -0.5) * BETA], axis=-1),
        'attn_w_o': normal((NA, D, D), D ** -0.5) * BETA,
        'attn_lam_q1': normal((NA, HEAD_DIM), 0.1),
        'attn_lam_k1': normal((NA, HEAD_DIM), 0.1),
        'attn_lam_q2': normal((NA, HEAD_DIM), 0.1),
        'attn_lam_k2': normal((NA, HEAD_DIM), 0.1),
        'attn_subln_g': gain((NA, 2 * HEAD_DIM)),
        'conv_w_pw1': normal((NC, D, 2 * D), D ** -0.5),
        'conv_b_pw1': normal((NC, 2 * D), 0.02),
        'conv_w_dw': normal((NC, CONV_WIDTH, D), CONV_WIDTH ** -0.5),
        'conv_b_dw': normal((NC, D), 0.02),
        'conv_ln_g': gain((NC, D)),
        'conv_ln_b': normal((NC, D), 0.02),
        'conv_w_pw2': normal((NC, D, D), D ** -0.5) * BETA,
        'conv_b_pw2': normal((NC, D), 0.02),
        'hy_w_in': normal((NH, D, 3 * D), D ** -0.5),
        'hy_b_in': normal((NH, 3 * D), 0.02),
        'hy_w_short': normal((NH, SHORT_WIDTH, 3 * D), SHORT_WIDTH ** -0.5),
        'hy_b_short': normal((NH, 3 * D), 0.02),
        'hy_f_w1': normal((NH, POS_EMB_DIM, FILTER_WIDTH), POS_EMB_DIM ** -0.5),
        'hy_f_b1': normal((NH, FILTER_WIDTH), 0.02),
        'hy_f_w2': normal((NH, FILTER_WIDTH, FILTER_WIDTH), FILTER_WIDTH ** -0.5),
        'hy_f_b2': normal((NH, FILTER_WIDTH), 0.02),
        'hy_f_w3': normal((NH, FILTER_WIDTH, FILTER_WIDTH), FILTER_WIDTH ** -0.5),
        'hy_f_b3': normal((NH, FILTER_WIDTH), 0.02),
        'hy_f_freq': gain((NH, FILTER_WIDTH)),
        'hy_f_wout': normal((NH, FILTER_WIDTH, HYENA_ORDER * 2 * D), 0.05 * FILTER_WIDTH ** -0.5),
        'hy_f_bias': normal((NH, HYENA_ORDER, D), 1.0),
        'hy_w_out': normal((NH, D, D), D ** -0.5) * BETA,
        'hy_b_out': normal((NH, D), 0.02),
        'ln1_g': gain((DEPTH, D)),
        'ln1_b': normal((DEPTH, D), 0.02),
        'ln2_g': gain((DEPTH, D)),
        'ln2_b': normal((DEPTH, D), 0.02),
        'moe_w_group': normal((DEPTH, D, N_GROUPS), D ** -0.5),
        'moe_b_group': normal((DEPTH, N_GROUPS), 0.01),
        'moe_w_expert': normal((DEPTH, D, N_EXPERTS), D ** -0.5),
        'moe_b_expert': normal((DEPTH, N_EXPERTS), 0.01),
        'moe_w_gate': normal((DEPTH, N_EXPERTS, D, EXPERT_FF), D ** -0.5),
        'moe_w_up': normal((DEPTH, N_EXPERTS, D, EXPERT_FF), D ** -0.5),
        'moe_w_down': normal((DEPTH, N_EXPERTS, EXPERT_FF, D), EXPERT_FF ** -0.5) * BETA,
        'ple_w_up': normal((DEPTH, PLE_DIM, D), PLE_DIM ** -0.5),
        'ple_w_gate': normal((DEPTH, D, D), D ** -0.5),
    }


def reference(x_prompt, x_sample, p_prompt, p_sample,
              attn_w_qkv, attn_w_o, attn_lam_q1, attn_lam_k1, attn_lam_q2, attn_lam_k2, attn_subln_g,
              conv_w_pw1, conv_b_pw1, conv_w_dw, conv_b_dw, conv_ln_g, conv_ln_b, conv_w_pw2, conv_b_pw2,
              hy_w_in, hy_b_in, hy_w_short, hy_b_short, hy_f_w1, hy_f_b1, hy_f_w2, hy_f_b2, hy_f_w3, hy_f_b3,
              hy_f_freq, hy_f_wout, hy_f_bias, hy_w_out, hy_b_out,
              ln1_g, ln1_b, ln2_g, ln2_b,
              moe_w_group, moe_b_group, moe_w_expert, moe_b_expert, moe_w_gate, moe_w_up, moe_w_down,
              ple_w_up, ple_w_gate):
    attn = (attn_w_qkv, attn_w_o, attn_lam_q1, attn_lam_k1, attn_lam_q2, attn_lam_k2, attn_subln_g)
    conv = (conv_w_pw1, conv_b_pw1, conv_w_dw, conv_b_dw, conv_ln_g, conv_ln_b, conv_w_pw2, conv_b_pw2)
    hyena = (hy_w_in, hy_b_in, hy_w_short, hy_b_short, hy_f_w1, hy_f_b1, hy_f_w2, hy_f_b2, hy_f_w3, hy_f_b3,
             hy_f_freq, hy_f_wout, hy_f_bias, hy_w_out, hy_b_out)
    norms = (ln1_g, ln1_b, ln2_g, ln2_b)
    moe = (moe_w_group, moe_b_group, moe_w_expert, moe_b_expert, moe_w_gate, moe_w_up, moe_w_down)
    ple = (ple_w_up, ple_w_gate)
    y_prompt = trunk(x_prompt, p_prompt, attn, conv, hyena, norms, moe, ple)
    y_sample = trunk(x_sample, p_sample, attn, conv, hyena, norms, moe, ple)
    return (y_prompt, y_sample)
```

```python
from contextlib import ExitStack
import numpy as np
import ml_dtypes
import concourse.bass as bass
import concourse.mybir as mybir
from concourse.bass_utils import run_bass_kernel_spmd

F32 = mybir.dt.float32
BF16 = mybir.dt.bfloat16
AF = mybir.ActivationFunctionType
ALU = mybir.AluOpType
AX = mybir.AxisListType

ENGS = ("pe", "act", "dve", "pool", "sp")


class Buf:
    __slots__ = ("name", "t", "w", "r", "sems", "cnt", "ps")

    def __init__(self, name, t=None, ps=False):
        self.name = name
        self.t = t
        self.ps = ps
        self.w = None
        self.r = []
        self.sems = None
        self.cnt = 0


class Op:
    __slots__ = ("eng", "fn", "deps", "sig", "is_dma", "key", "final", "sem", "val", "idx")


class Sched:
    SEM_LIMIT = 24000

    def __init__(self, nc):
        self.nc = nc
        self.stack = ExitStack()
        self.ops = []
        self._dbufs = {}

    def sbuf(self, name, shape, dtype):
        return Buf(name, self.stack.enter_context(self.nc.sbuf_tensor(name, list(shape), dtype)))

    def psum(self, name, shape, dtype):
        return Buf(name, self.stack.enter_context(self.nc.psum_tensor(name, list(shape), dtype)), ps=True)

    def dbuf(self, name):
        b = self._dbufs.get(name)
        if b is None:
            b = self._dbufs[name] = Buf(name)
        return b

    def _add(self, eng, fn, reads, writes, is_dma, key, final):
        op = Op()
        op.eng = eng
        op.fn = fn
        op.sig = final or is_dma
        op.is_dma = is_dma
        op.key = key
        op.final = final
        op.sem = None
        op.val = 0
        op.idx = len(self.ops)
        deps = {}
        for b in reads:
            if b.w is not None:
                deps[b.w.idx] = b.w
            if b.ps:
                for r in b.r:
                    if r.eng != eng:
                        deps[r.idx] = r
        for b in writes:
            if b.w is not None:
                deps[b.w.idx] = b.w
            for r in b.r:
                deps[r.idx] = r
        deps.pop(op.idx, None)
        for b in reads:
            b.r.append(op)
        for b in writes:
            b.w = op
            b.r = []
        dl = []
        for d in deps.values():
            if d.eng == "pe" and eng == "pe" and not is_dma and not d.is_dma:
                continue
            d.sig = True
            dl.append(d)
        op.deps = dl
        self.ops.append(op)
        return op

    def op(self, eng, fn, reads=(), writes=()):
        return self._add(eng, fn, reads, writes, False, None, False)

    def dma(self, eng, out, in_, reads=(), writes=(), final=False, key=None, **kw):
        if key is None:
            for b in list(writes) + list(reads):
                if b.t is not None:
                    key = b
                    break
            else:
                key = (list(writes) + list(reads))[0]
        return self._add(eng, lambda e: e.dma_start(out=out, in_=in_, **kw), reads, writes, True, key, final)

    def emit(self):
        nc = self.nc
        stack = self.stack
        n_sem = [0]

        def newsem():
            n_sem[0] += 1
            return stack.enter_context(nc.semaphore("s%d" % n_sem[0]))

        eng_sems = {e: [] for e in ENGS}
        eng_cnt = {e: 0 for e in ENGS}
        lim = self.SEM_LIMIT
        for op in self.ops:
            if not op.sig:
                continue
            if op.is_dma:
                b = op.key
                if b.sems is None:
                    b.sems = []
                k = b.cnt
                b.cnt += 1
                per = lim // 16
                while len(b.sems) <= k // per:
                    b.sems.append(newsem())
                op.sem = b.sems[k // per]
                op.val = 16 * (k % per + 1)
            else:
                k = eng_cnt[op.eng]
                eng_cnt[op.eng] += 1
                sl = eng_sems[op.eng]
                while len(sl) <= k // lim:
                    sl.append(newsem())
                op.sem = sl[k // lim]
                op.val = k % lim + 1
        queues = {e: [] for e in ENGS}
        for op in self.ops:
            queues[op.eng].append(op)
        finals = [op for op in self.ops if op.final]
        self.n_sems = n_sem[0]

        def run_queue(e, ops, last):
            known = {}
            for op in ops:
                need = {}
                for d in op.deps:
                    sid = id(d.sem)
                    if known.get(sid, 0) >= d.val:
                        continue
                    cur = need.get(sid)
                    if cur is None or cur[1] < d.val:
                        need[sid] = (d.sem, d.val)
                for sid, (sem, val) in need.items():
                    e.wait_ge(sem, val)
                    known[sid] = val
                ins = op.fn(e)
                if op.sig:
                    ins.then_inc(op.sem, 16 if op.is_dma else 1)
            if last:
                for op in finals:
                    e.wait_ge(op.sem, op.val)

        with nc.Block() as block:
            @block.tensor
            def _(e):
                run_queue(e, queues["pe"], False)

            @block.scalar
            def _(e):
                run_queue(e, queues["act"], False)

            @block.vector
            def _(e):
                run_queue(e, queues["dve"], False)

            @block.gpsimd
            def _(e):
                run_queue(e, queues["pool"], False)

            @block.sync
            def _(e):
                run_queue(e, queues["sp"], True)
        stack.close()


D = 1024
ALPHA = 8 ** 0.25
LN_EPS = 1e-5
NEG = -1.0e30


def bcast_rows(dt, off, n, parts=128):
    return bass.AP(dt, off, [[0, parts], [1, n]])


class Ctx:
    def __init__(self, nc, S):
        self.nc = nc
        self.S = S
        self.uid = 0

    def din(self, name, shape, dtype=F32):
        return self.nc.dram_tensor(name, list(shape), dtype, kind="ExternalInput")

    def dout(self, name, shape, dtype=F32):
        return self.nc.dram_tensor(name, list(shape), dtype, kind="ExternalOutput")


def layer_norm_tile(S, src, dst, g_bc, b_bc, scr):
    st, mv, tmp = scr
    for j in range(2):
        S.op("dve", lambda e, j=j: e.bn_stats(st.t[:, j * 6:(j + 1) * 6], src.t[:, j * 512:(j + 1) * 512]),
             reads=[src], writes=[st])
    S.op("dve", lambda e: e.bn_aggr(mv.t[:, 0:2], st.t[:, 0:12]), reads=[st], writes=[mv])
    S.op("dve", lambda e: e.tensor_scalar(mv.t[:, 2:3], mv.t[:, 1:2], LN_EPS, None, ALU.add), reads=[mv], writes=[mv])
    S.op("act", lambda e: e.activation(mv.t[:, 3:4], mv.t[:, 2:3], AF.Sqrt), reads=[mv], writes=[mv])
    S.op("dve", lambda e: e.reciprocal(mv.t[:, 4:5], mv.t[:, 3:4]), reads=[mv], writes=[mv])
    S.op("dve", lambda e: e.tensor_scalar(tmp.t[:], src.t[:], mv.t[:, 0:1], mv.t[:, 4:5], ALU.subtract, ALU.mult),
         reads=[src, mv], writes=[tmp])
    S.op("dve", lambda e: e.tensor_tensor(tmp.t[:], tmp.t[:], g_bc.t[:], ALU.mult), reads=[tmp, g_bc], writes=[tmp])
    S.op("dve", lambda e: e.tensor_tensor(dst.t[:], tmp.t[:], b_bc.t[:], ALU.add), reads=[tmp, b_bc], writes=[dst])


def transpose_tile(S, src_ap_fn, src_buf, ident, pst, nk, evac):
    for g in range(0, nk, 4):
        n = min(4, nk - g)
        for j in range(n):
            S.op("pe", lambda e, g=g, j=j: e.transpose(pst.t[:, j * 128:(j + 1) * 128], src_ap_fn(g + j), ident.t[:]),
                 reads=[src_buf, ident], writes=[pst])
        evac(g, n)


def build_tok(T):
    nc = bass.Bass("TRN2", target_bir_lowering=False)
    S = Sched(nc)
    C = Ctx(nc, S)
    NB = T // 512
    x_d = C.din("x", [T, D])
    h_d = C.din("h", [T, D])
    p_d = C.din("p", [T, 256])
    vec_d = C.din("vecs", [4, D])
    wr_d = C.din("w_router", [D, 20])
    br_d = C.din("b_router", [20])
    wg_d = C.din("w_gate", [16, D, 512])
    wu_d = C.din("w_up", [16, D, 512])
    wd_d = C.din("w_down", [16, 512, D])
    pu_d = C.din("ple_up", [256, D])
    pg_d = C.din("ple_gate", [D, D])
    id_d = C.din("identd", [128, 128])
    o_d = C.dout("xo", [T, D])

    ident = S.sbuf("ident", [128, 128], F32)
    S.dma("sp", ident.t[:], id_d.ap(), writes=[ident])
    vb = []
    for i in range(4):
        b = S.sbuf("vec%d" % i, [128, D], F32)
        S.dma("sp", b.t[:], bcast_rows(vec_d, i * D, D), writes=[b])
        vb.append(b)
    wr = S.sbuf("wr", [128, 8, 20], F32)
    S.dma("sp", wr.t[:], wr_d.ap().rearrange("(k p) n -> p k n", p=128), writes=[wr])
    brb = S.sbuf("brb", [128, 20], F32)
    S.dma("sp", brb.t[:], bcast_rows(br_d, 0, 20), writes=[brb])
    stg = [S.sbuf("stg%d" % i, [128, 8, 512], F32) for i in range(2)]
    pgw = S.sbuf("pgw", [128, 8, D], BF16)
    puw = S.sbuf("puw", [128, 2, D], BF16)
    for hf in range(2):
        S.dma("sp", stg[hf].t[:], pg_d.ap()[:, hf * 512:(hf + 1) * 512].rearrange("(k p) n -> p k n", p=128), writes=[stg[hf]])
        S.op("pool", lambda e, hf=hf: e.tensor_copy(pgw.t[:, :, hf * 512:(hf + 1) * 512], stg[hf].t[:]), reads=[stg[hf]], writes=[pgw])
    for hf in range(2):
        S.dma("sp", stg[hf].t[:, 0:2, :], pu_d.ap()[:, hf * 512:(hf + 1) * 512].rearrange("(k p) n -> p k n", p=128), writes=[stg[hf]])
        S.op("pool", lambda e, hf=hf: e.tensor_copy(puw.t[:, :, hf * 512:(hf + 1) * 512], stg[hf].t[:, 0:2, :]), reads=[stg[hf]], writes=[puw])

    xin = [S.sbuf("xin%d" % i, [128, D], F32) for i in range(2)]
    hin = [S.sbuf("hin%d" % i, [128, D], F32) for i in range(2)]
    pin = [S.sbuf("pin%d" % i, [128, 256], F32) for i in range(2)]
    x1t = S.sbuf("x1t", [128, D], F32)
    lnscr = (S.sbuf("lnst", [128, 12], F32), S.sbuf("lnmv", [128, 8], F32), S.sbuf("lntmp", [128, D], F32))
    acc = S.sbuf("acc", [128, 4, D], F32)
    accb = [Buf("acc%d" % i) for i in range(4)]
    xT = S.sbuf("xT", [128, 8, 512], BF16)
    xTf = S.sbuf("xTf", [128, 8, 128], F32)
    pT = S.sbuf("pT", [128, 2, 512], BF16)
    pTf = S.sbuf("pTf", [128, 2, 128], F32)
    lg = S.sbuf("lg", [128, 20], F32)
    rt = S.sbuf("rt", [128, 96], F32)
    comb = S.sbuf("comb", [128, 4, 16], F32)
    wgb = [S.sbuf("wgb%d" % i, [128, 8, 512], BF16) for i in range(2)]
    wub = [S.sbuf("wub%d" % i, [128, 8, 512], BF16) for i in range(2)]
    wdb = [S.sbuf("wdb%d" % i, [128, 4, D], BF16) for i in range(2)]
    sg = [S.sbuf("sg%d" % i, [128, 512], F32) for i in range(2)]
    hid = [S.sbuf("hid%d" % i, [128, 4, 512], BF16) for i in range(2)]
    osb = [S.sbuf("osb%d" % i, [128, 512], F32) for i in range(2)]
    pst = S.psum("pst", [128, 512], F32)
    psr = S.psum("psr", [128, 512], F32)
    psg = [S.psum("psg%d" % i, [128, 512], F32) for i in range(2)]
    psu = [S.psum("psu%d" % i, [128, 512], F32) for i in range(2)]
    psd = [S.psum("psd%d" % i, [128, 512], F32) for i in range(2)]
    wcnt = [0]

    def load_w(dst, src_ap, nk):
        st = stg[wcnt[0] % 2]
        wcnt[0] += 1
        n = src_ap.shape[-1]
        if n == 512:
            S.dma("sp", st.t[:, 0:nk, :], src_ap.rearrange("(k p) n -> p k n", p=128), writes=[st])
            S.op("pool", lambda e: e.tensor_copy(dst.t[:, 0:nk, :], st.t[:, 0:nk, :]), reads=[st], writes=[dst])
        else:
            stv = st.t[:].rearrange("p a b -> p (a b)").rearrange("p (k n) -> p k n", k=nk)
            S.dma("sp", stv, src_ap.rearrange("(k p) n -> p k n", p=128), writes=[st])
            S.op("pool", lambda e: e.tensor_copy(dst.t[:, 0:nk, :], stv), reads=[st], writes=[dst])

    for blk in range(NB):
        t0 = blk * 512
        for tt in range(4):
            r0 = t0 + tt * 128
            xi, hi = xin[tt % 2], hin[tt % 2]
            S.dma("sp", xi.t[:], x_d.ap()[r0:r0 + 128, :], writes=[xi])
            S.dma("sp", hi.t[:], h_d.ap()[r0:r0 + 128, :], writes=[hi])
            S.op("dve", lambda e, xi=xi, hi=hi: e.scalar_tensor_tensor(xi.t[:], xi.t[:], ALPHA, hi.t[:], ALU.mult, ALU.add),
                 reads=[xi, hi], writes=[xi])
            layer_norm_tile(S, xi, x1t, vb[0], vb[1], lnscr)
            S.op("act", lambda e, tt=tt: e.mul(acc.t[:, tt, :], x1t.t[:], ALPHA), reads=[x1t], writes=[accb[tt]])

            def evac(g, n, tt=tt):
                S.op("act", lambda e: e.activation(xTf.t[:, g:g + n, :], pst.t[:, 0:n * 128].rearrange("p (a b) -> p a b", a=n), AF.Copy),
                     reads=[pst], writes=[xTf])
                S.op("pool", lambda e: e.tensor_copy(xT.t[:, g:g + n, tt * 128:(tt + 1) * 128], xTf.t[:, g:g + n, :]),
                     reads=[xTf], writes=[xT])
            transpose_tile(S, lambda k: x1t.t[:, k * 128:(k + 1) * 128], x1t, ident, pst, 8, evac)
            for kc in range(8):
                S.op("pe", lambda e, kc=kc: e.matmul(psr.t[:, 0:20], xTf.t[:, kc, :], wr.t[:, kc, :], start=(kc == 0), stop=(kc == 7)),
                     reads=[xTf, wr], writes=[psr])
            S.op("dve", lambda e: e.tensor_tensor(lg.t[:], psr.t[:, 0:20], brb.t[:], ALU.add), reads=[psr, brb], writes=[lg])
            R = rt.t
            dv = lambda fn, rd=(rt, lg), wrb=(rt,): S.op("dve", fn, reads=list(rd), writes=list(wrb))
            dv(lambda e: e.tensor_reduce(R[:, 0:1], lg.t[:, 0:4], AX.X, ALU.max))
            dv(lambda e: e.tensor_scalar(R[:, 4:8], lg.t[:, 0:4], R[:, 0:1], None, ALU.is_equal))
            dv(lambda e: e.tensor_scalar(R[:, 1:2], R[:, 0:1], -1.0, None, ALU.mult))
            S.op("act", lambda e: e.activation(R[:, 8:12], lg.t[:, 0:4], AF.Exp, bias=R[:, 1:2], scale=1.0),
                 reads=[rt, lg], writes=[rt])
            dv(lambda e: e.tensor_reduce(R[:, 2:3], R[:, 8:12], AX.X, ALU.add))
            dv(lambda e: e.reciprocal(R[:, 3:4], R[:, 2:3]))
            dv(lambda e: e.tensor_scalar(R[:, 12:16], R[:, 4:8], 1.0, -NEG, ALU.subtract, ALU.mult))
            for g in range(4):
                dv(lambda e, g=g: e.tensor_scalar(R[:, 16 + 4 * g:20 + 4 * g], lg.t[:, 4 + 4 * g:8 + 4 * g],
                                                  R[:, 12 + g:13 + g], None, ALU.add))
            dv(lambda e: e.max(R[:, 32:40], R[:, 16:32]))
            dv(lambda e: e.tensor_scalar(R[:, 40:56], R[:, 16:32], R[:, 32:33], None, ALU.is_equal))
            dv(lambda e: e.tensor_scalar(R[:, 56:72], R[:, 16:32], R[:, 33:34], None, ALU.is_equal))
            dv(lambda e: e.tensor_tensor(R[:, 72:73], R[:, 32:33], R[:, 33:34], ALU.subtract))
            S.op("act", lambda e: e.activation(R[:, 73:74], R[:, 72:73], AF.Sigmoid), reads=[rt], writes=[rt])
            dv(lambda e: e.tensor_tensor(R[:, 74:75], R[:, 73:74], R[:, 3:4], ALU.mult))
            dv(lambda e: e.tensor_tensor(R[:, 75:76], R[:, 3:4], R[:, 74:75], ALU.subtract))
            dv(lambda e: e.tensor_scalar(R[:, 76:92], R[:, 40:56], R[:, 74:75], None, ALU.mult))
            S.op("dve", lambda e, tt=tt: e.scalar_tensor_tensor(comb.t[:, tt, :], R[:, 56:72], R[:, 75:76], R[:, 76:92], ALU.mult, ALU.add),
                 reads=[rt], writes=[comb])
        for ex in range(16):
            wg, wu, wd = wgb[ex % 2], wub[ex % 2], wdb[ex % 2]
            load_w(wg, wg_d.ap()[ex], 8)
            load_w(wu, wu_d.ap()[ex], 8)
            load_w(wd, wd_d.ap()[ex], 4)
            hb = hid[ex % 2]
            for fc in range(4):
                pg_, pu_, sgb = psg[fc % 2], psu[fc % 2], sg[fc % 2]
                for kc in range(8):
                    S.op("pe", lambda e, kc=kc, fc=fc, pg_=pg_, wg=wg: e.matmul(pg_.t[:], wg.t[:, kc, fc * 128:(fc + 1) * 128], xT.t[:, kc, :],
                                                                               start=(kc == 0), stop=(kc == 7)), reads=[wg, xT], writes=[pg_])
                for kc in range(8):
                    S.op("pe", lambda e, kc=kc, fc=fc, pu_=pu_, wu=wu: e.matmul(pu_.t[:], wu.t[:, kc, fc * 128:(fc + 1) * 128], xT.t[:, kc, :],
                                                                               start=(kc == 0), stop=(kc == 7)), reads=[wu, xT], writes=[pu_])
                S.op("act", lambda e, pg_=pg_, sgb=sgb: e.activation(sgb.t[:], pg_.t[:], AF.Silu), reads=[pg_], writes=[sgb])
                S.op("dve", lambda e, fc=fc, pu_=pu_, sgb=sgb, hb=hb: e.tensor_tensor(hb.t[:, fc, :], sgb.t[:], pu_.t[:], ALU.mult),
                     reads=[sgb, pu_], writes=[hb])
            for tt in range(4):
                for hf in range(2):
                    pd = psd[(tt * 2 + hf) % 2]
                    for fc in range(4):
                        S.op("pe", lambda e, fc=fc, tt=tt, hf=hf, pd=pd, hb=hb, wd=wd: e.matmul(
                            pd.t[:], hb.t[:, fc, tt * 128:(tt + 1) * 128], wd.t[:, fc, hf * 512:(hf + 1) * 512],
                            start=(fc == 0), stop=(fc == 3)), reads=[hb, wd], writes=[pd])
                    S.op("dve", lambda e, tt=tt, hf=hf, pd=pd, ex=ex: e.scalar_tensor_tensor(
                        acc.t[:, tt, hf * 512:(hf + 1) * 512], pd.t[:], comb.t[:, tt, ex:ex + 1], acc.t[:, tt, hf * 512:(hf + 1) * 512],
                        ALU.mult, ALU.add), reads=[pd, comb, accb[tt]], writes=[accb[tt]])
        for tt in range(4):
            r0 = t0 + tt * 128
            a_view = Buf("accv")
            a_view.t = acc.t[:, tt, :]
            src = accb[tt]
            src.t = acc.t[:, tt, :]
            layer_norm_tile(S, src, src, vb[2], vb[3], lnscr)
            pi = pin[tt % 2]
            S.dma("sp", pi.t[:], p_d.ap()[r0:r0 + 128, :], writes=[pi])

            def evac2(g, n, tt=tt):
                S.op("act", lambda e: e.activation(xTf.t[:, g:g + n, :], pst.t[:, 0:n * 128].rearrange("p (a b) -> p a b", a=n), AF.Copy),
                     reads=[pst], writes=[xTf])
                S.op("pool", lambda e: e.tensor_copy(xT.t[:, g:g + n, tt * 128:(tt + 1) * 128], xTf.t[:, g:g + n, :]),
                     reads=[xTf], writes=[xT])
            transpose_tile(S, lambda k, tt=tt: acc.t[:, tt, k * 128:(k + 1) * 128], src, ident, pst, 8, evac2)

            def evac3(g, n, tt=tt):
                S.op("act", lambda e: e.activation(pTf.t[:, g:g + n, :], pst.t[:, 0:n * 128].rearrange("p (a b) -> p a b", a=n), AF.Copy),
                     reads=[pst], writes=[pTf])
                S.op("pool", lambda e: e.tensor_copy(pT.t[:, g:g + n, tt * 128:(tt + 1) * 128], pTf.t[:, g:g + n, :]),
                     reads=[pTf], writes=[pT])
            transpose_tile(S, lambda k, pi=pi: pi.t[:, k * 128:(k + 1) * 128], pi, ident, pst, 2, evac3)
            for hf in range(2):
                pg_, pu_, sgb, ob = psg[hf], psu[hf], sg[hf], osb[hf]
                for kc in range(8):
                    S.op("pe", lambda e, kc=kc, tt=tt, hf=hf, pg_=pg_: e.matmul(pg_.t[:], xT.t[:, kc, tt * 128:(tt + 1) * 128],
                                                                              pgw.t[:, kc, hf * 512:(hf + 1) * 512], start=(kc == 0), stop=(kc == 7)),
                         reads=[xT, pgw], writes=[pg_])
                for kc in range(2):
                    S.op("pe", lambda e, kc=kc, tt=tt, hf=hf, pu_=pu_: e.matmul(pu_.t[:], pT.t[:, kc, tt * 128:(tt + 1) * 128],
                                                                              puw.t[:, kc, hf * 512:(hf + 1) * 512], start=(kc == 0), stop=(kc == 1)),
                         reads=[pT, puw], writes=[pu_])
                S.op("act", lambda e, pg_=pg_, sgb=sgb: e.activation(sgb.t[:], pg_.t[:], AF.Sigmoid), reads=[pg_], writes=[sgb])
                S.op("dve", lambda e, pu_=pu_, sgb=sgb: e.tensor_tensor(sgb.t[:], sgb.t[:], pu_.t[:], ALU.mult), reads=[sgb, pu_], writes=[sgb])
                S.op("dve", lambda e, tt=tt, hf=hf, sgb=sgb, ob=ob: e.tensor_tensor(ob.t[:], sgb.t[:], acc.t[:, tt, hf * 512:(hf + 1) * 512], ALU.add),
                     reads=[sgb, src], writes=[ob])
                S.dma("sp", o_d.ap()[r0:r0 + 128, hf * 512:(hf + 1) * 512], ob.t[:], reads=[ob], writes=[S.dbuf("xo%d_%d" % (r0, hf))], final=True)
    S.emit()
    return nc


def tok_weights(ln1_g, ln1_b, ln2_g, ln2_b, w_group, b_group, w_expert, b_expert, w_gate, w_up, w_down, ple_up, ple_gate):
    return dict(
        vecs=np.ascontiguousarray(np.stack([ln1_g, ln1_b, ln2_g, ln2_b]).astype(np.float32)),
        w_router=np.ascontiguousarray(np.concatenate([w_group, w_expert], axis=1)),
        b_router=np.ascontiguousarray(np.concatenate([b_group, b_expert])),
        w_gate=np.ascontiguousarray(w_gate), w_up=np.ascontiguousarray(w_up), w_down=np.ascontiguousarray(w_down),
        ple_up=np.ascontiguousarray(ple_up), ple_gate=np.ascontiguousarray(ple_gate),
        identd=np.eye(128, dtype=np.float32))


def build_attn(LP, LS, NS):
    nc = bass.Bass("TRN2", target_bir_lowering=False)
    S = Sched(nc)
    C = Ctx(nc, S)
    xp_d = C.din("xp", [LP, D])
    xs_d = C.din("xs", [NS, LS, D])
    wh_d = C.din("w_heads", [9, D, 384])
    lam_d = C.din("lamv", [4, 64])
    g_d = C.din("subg", [128])
    li_d = C.din("laminit", [2])
    rl_d = C.din("rl", [128, 512])
    td_d = C.din("td", [128, 4 * 512])
    hsc_d = C.din("hsc", [128, 9 * 130])
    id_d = C.din("identd", [128, 128])
    op_d = C.dout("hp", [128, LP])
    os_d = C.dout("hs", [NS, D, LS])
    LM = max(LP, LS)

    def cload(name, shape, src):
        b = S.sbuf(name + "_sb", shape, F32)
        S.dma("sp", b.t[:], src, writes=[b])
        return b
    ident = cload("ident", [128, 128], id_d.ap())
    rl = cload("rl", [128, 512], rl_d.ap())
    td = cload("td", [128, 2048], td_d.ap())
    hsc = cload("hsc", [128, 9 * 130], hsc_d.ap())
    lamv = cload("lamv", [128, 256], bcast_rows(lam_d, 0, 256))
    lin = cload("lin", [128, 2], bcast_rows(li_d, 0, 2))
    gcol = S.sbuf("gcol", [128, 1], F32)
    S.dma("sp", gcol.t[:], bass.AP(g_d, 0, [[1, 128], [1, 1]]), writes=[gcol])
    onesb = S.sbuf("onesb", [128, 128], BF16)
    onesf = S.sbuf("onesf", [128, 128], F32)
    S.op("dve", lambda e: e.memset(onesb.t[:], 1.0), writes=[onesb])
    S.op("dve", lambda e: e.memset(onesf.t[:], 1.0 / 128.0), writes=[onesf])
    lw = S.sbuf("lw", [128, 136], F32)
    S.op("dve", lambda e: e.tensor_tensor(lw.t[:, 0:64], lamv.t[:, 0:64], lamv.t[:, 64:128], ALU.mult), reads=[lamv], writes=[lw])
    S.op("dve", lambda e: e.tensor_tensor(lw.t[:, 64:128], lamv.t[:, 128:192], lamv.t[:, 192:256], ALU.mult), reads=[lamv, lw], writes=[lw])
    S.op("dve", lambda e: e.tensor_reduce(lw.t[:, 128:129], lw.t[:, 0:64], AX.X, ALU.add), reads=[lw], writes=[lw])
    S.op("dve", lambda e: e.tensor_reduce(lw.t[:, 129:130], lw.t[:, 64:128], AX.X, ALU.add), reads=[lw], writes=[lw])
    S.op("act", lambda e: e.activation(lw.t[:, 130:132], lw.t[:, 128:130], AF.Exp), reads=[lw], writes=[lw])
    S.op("dve", lambda e: e.tensor_tensor(lw.t[:, 132:133], lw.t[:, 131:132], lw.t[:, 130:131], ALU.subtract), reads=[lw], writes=[lw])
    S.op("dve", lambda e: e.tensor_tensor(lw.t[:, 133:134], lw.t[:, 132:133], lin.t[:, 0:1], ALU.subtract), reads=[lw, lin], writes=[lw])
    S.op("dve", lambda e: e.tensor_tensor(lw.t[:, 134:135], gcol.t[:], lin.t[:, 1:2], ALU.mult), reads=[lw, lin, gcol], writes=[lw])
    nlam = lw.t[:, 133:134]
    gsc = lw.t[:, 134:135]

    stg = S.sbuf("stg", [128, 8, 384], F32)
    wh = S.sbuf("wh", [128, 8, 384], BF16)
    xin = [S.sbuf("xin%d" % i, [128, D], F32) for i in range(2)]
    xT = S.sbuf("xT", [128, 8, LS], BF16)
    qT = S.sbuf("qT", [128, LM], BF16)
    kT = S.sbuf("kT", [128, LM], BF16)
    vv = S.sbuf("vv", [128, LM // 128, 128], BF16)
    tmp = [S.sbuf("tmp%d" % i, [128, 512], F32) for i in range(2)]
    Eb = [S.sbuf("E%d" % i, [128, 512], BF16) for i in range(2)]
    oc = [S.sbuf("oc%d" % i, [128, 512], F32) for i in range(2)]
    rs = S.sbuf("rs", [128, 512], F32)
    ob = [S.sbuf("ob%d" % i, [128, 512], F32) for i in range(2)]
    sq = S.sbuf("sq", [128, 512], F32)
    pst = S.psum("pst", [128, 512], F32)
    psp = [S.psum("psp%d" % i, [128, 512], F32) for i in range(2)]
    pss = [S.psum("pss%d" % i, [128, 512], F32) for i in range(2)]
    pso = S.psum("pso", [128, 512], F32)
    psn = S.psum("psn", [128, 512], F32)
    psm = S.psum("psm", [128, 512], F32)
    cnt = [0]

    def load_heads(hs):
        S.dma("sp", stg.t[:], wh_d.ap()[hs].rearrange("(k p) n -> p k n", p=128), writes=[stg])
        S.op("pool", lambda e: e.tensor_copy(wh.t[:], stg.t[:]), reads=[stg], writes=[wh])

    def transpose_block(src_rows_ap, c0):
        for j in range(4):
            xi = xin[j % 2]
            S.dma("sp", xi.t[:], src_rows_ap[j * 128:(j + 1) * 128, :], writes=[xi])
            for g in range(0, 8, 4):
                for jj in range(4):
                    S.op("pe", lambda e, g=g, jj=jj, xi=xi: e.transpose(pst.t[:, jj * 128:(jj + 1) * 128], xi.t[:, (g + jj) * 128:(g + jj + 1) * 128], ident.t[:]),
                         reads=[xi, ident], writes=[pst])
                S.op("act", lambda e, g=g, j=j: e.activation(xT.t[:, g:g + 4, c0 + j * 128:c0 + (j + 1) * 128],
                                                            pst.t[:].rearrange("p (a b) -> p a b", a=4), AF.Copy), reads=[pst], writes=[xT])

    def project(c0, t0):
        for which, dst in ((0, qT), (1, kT)):
            pp = psp[which]
            for kc in range(8):
                S.op("pe", lambda e, kc=kc, which=which, pp=pp: e.matmul(pp.t[:], wh.t[:, kc, which * 128:(which + 1) * 128], xT.t[:, kc, c0:c0 + 512],
                                                                       start=(kc == 0), stop=(kc == 7)), reads=[wh, xT], writes=[pp])
            S.op("act", lambda e, pp=pp, dst=dst: e.activation(dst.t[:, t0:t0 + 512], pp.t[:], AF.Copy), reads=[pp], writes=[dst])
        for j in range(4):
            for kc in range(8):
                S.op("pe", lambda e, kc=kc, j=j: e.matmul(pst.t[:, j * 128:(j + 1) * 128], xT.t[:, kc, c0 + j * 128:c0 + (j + 1) * 128], wh.t[:, kc, 256:384],
                                                        start=(kc == 0), stop=(kc == 7)), reads=[wh, xT], writes=[pst])
        S.op("act", lambda e: e.activation(vv.t[:, t0 // 128:t0 // 128 + 4, :], pst.t[:].rearrange("p (a b) -> p a b", a=4), AF.Copy),
             reads=[pst], writes=[vv])

    def attend(L, hs, out_ap_fn, out_name):
        nk = L // 128
        H = hsc.t
        hb = hs * 130
        for qi in range(L // 512):
            for c in range(2):
                for kt in range(nk):
                    dl = kt - 4 * qi
                    i = cnt[0] % 2
                    cnt[0] += 1
                    ps_, tm, E = pss[i], tmp[i], Eb[i]
                    S.op("pe", lambda e, c=c, kt=kt, qi=qi, ps_=ps_: e.matmul(ps_.t[:], kT.t[c * 64:(c + 1) * 64, kt * 128:(kt + 1) * 128],
                                                                            qT.t[c * 64:(c + 1) * 64, qi * 512:(qi + 1) * 512], start=True, stop=True),
                         reads=[kT, qT], writes=[ps_])
                    if 0 <= dl <= 3:
                        tab, sc, bc = td.t[:, dl * 512:(dl + 1) * 512], H[:, hb:hb + 1], H[:, hb + 2:hb + 3]
                    elif dl < 0:
                        tab, sc, bc = rl.t[:], H[:, hb:hb + 1], H[:, hb + 2 - dl:hb + 3 - dl]
                    else:
                        tab, sc, bc = rl.t[:], H[:, hb + 1:hb + 2], H[:, hb + 2 + dl:hb + 3 + dl]
                    S.op("dve", lambda e, tab=tab, sc=sc, ps_=ps_, tm=tm: e.scalar_tensor_tensor(tm.t[:], tab, sc, ps_.t[:], ALU.mult, ALU.add),
                         reads=[td, rl, hsc, ps_], writes=[tm])
                    S.op("act", lambda e, tm=tm, E=E, bc=bc: e.activation(E.t[:], tm.t[:], AF.Exp, bias=bc, scale=0.125), reads=[tm, hsc], writes=[E])
                    S.op("pe", lambda e, kt=kt, E=E: e.matmul(pso.t[:], vv.t[:, kt, :], E.t[:], start=(kt == 0), stop=(kt == nk - 1)), reads=[vv, E], writes=[pso])
                    S.op("pe", lambda e, kt=kt, E=E: e.matmul(psn.t[:], onesb.t[:], E.t[:], start=(kt == 0), stop=(kt == nk - 1)), reads=[onesb, E], writes=[psn])
                S.op("dve", lambda e: e.reciprocal(rs.t[:], psn.t[:]), reads=[psn], writes=[rs])
                S.op("dve", lambda e, c=c: e.tensor_tensor(oc[c].t[:], pso.t[:], rs.t[:], ALU.mult), reads=[pso, rs], writes=[oc[c]])
            o = ob[qi % 2]
            S.op("dve", lambda e, o=o: e.scalar_tensor_tensor(o.t[:], oc[1].t[:], nlam, oc[0].t[:], ALU.mult, ALU.add), reads=[oc[0], oc[1], lw], writes=[o])
            S.op("dve", lambda e, o=o: e.tensor_tensor(sq.t[:], o.t[:], o.t[:], ALU.mult), reads=[o], writes=[sq])
            S.op("pe", lambda e: e.matmul(psm.t[:], onesf.t[:], sq.t[:], start=True, stop=True), reads=[onesf, sq], writes=[psm])
            S.op("dve", lambda e: e.tensor_scalar(sq.t[:], psm.t[:], LN_EPS, None, ALU.add), reads=[psm], writes=[sq])
            S.op("act", lambda e: e.activation(sq.t[:], sq.t[:], AF.Sqrt), reads=[sq], writes=[sq])
            S.op("dve", lambda e: e.reciprocal(sq.t[:], sq.t[:]), reads=[sq], writes=[sq])
            S.op("dve", lambda e, o=o: e.tensor_tensor(o.t[:], o.t[:], sq.t[:], ALU.mult), reads=[o, sq], writes=[o])
            S.op("dve", lambda e, o=o: e.tensor_scalar(o.t[:], o.t[:], gsc, None, ALU.mult), reads=[o, lw], writes=[o])
            S.dma("sp", out_ap_fn(qi), o.t[:], reads=[o], writes=[S.dbuf("%s_%d" % (out_name, qi))], final=True)

    load_heads(8)
    for blk in range(LP // 512):
        transpose_block(xp_d.ap()[blk * 512:(blk + 1) * 512, :], 0)
        project(0, blk * 512)
    attend(LP, 8, lambda qi: op_d.ap()[:, qi * 512:(qi + 1) * 512], "hp")
    for s in range(NS):
        for blk in range(LS // 512):
            transpose_block(xs_d.ap()[s, blk * 512:(blk + 1) * 512, :], blk * 512)
        for h in range(8):
            load_heads(h)
            for blk in range(LS // 512):
                project(blk * 512, blk * 512)
            attend(LS, h, lambda qi, s=s, h=h: os_d.ap()[s, h * 128:(h + 1) * 128, qi * 512:(qi + 1) * 512], "hs%d_%d" % (s, h))
    S.emit()
    return nc


def attn_consts(core, lambda_init):
    p = np.arange(128, dtype=np.float32)[:, None]
    f = np.arange(512, dtype=np.float32)[None, :]
    rl = (f - p).astype(np.float32)
    td = np.stack([np.abs(f - p - 128.0 * j) for j in range(4)], axis=1).reshape(128, 2048).astype(np.float32)
    slopes = 2.0 ** (-8.0 * (np.arange(8, dtype=np.float64) + 1.0) / 8)
    hsc = np.zeros((9, 130), np.float64)
    for s in range(9):
        m = slopes[s] if s < 8 else slopes[core]
        hsc[s, 0] = -8.0 * m
        hsc[s, 1] = 8.0 * m
        hsc[s, 2:] = -m * 128.0 * np.arange(128)
    hsc = np.broadcast_to(hsc.reshape(1, -1), (128, 9 * 130)).astype(np.float32)
    return dict(rl=rl, td=td, hsc=np.ascontiguousarray(hsc), identd=np.eye(128, dtype=np.float32),
                laminit=np.array([lambda_init, 1.0 - lambda_init], np.float32))


def attn_heads(w_qkv, core):
    hs = list(range(8)) + [core]
    return np.ascontiguousarray(np.stack([
        np.concatenate([w_qkv[:, h * 128:(h + 1) * 128], w_qkv[:, 1024 + h * 128:1024 + (h + 1) * 128],
                        w_qkv[:, 2048 + h * 128:2048 + (h + 1) * 128]], axis=1) for h in hs]))


def attn_heads(w_qkv, core):
    hs = list(range(8)) + [core]
    return np.ascontiguousarray(np.stack([
        np.concatenate([w_qkv[:, h * 128:(h + 1) * 128], w_qkv[:, 1024 + h * 128:1024 + (h + 1) * 128],
                        w_qkv[:, 2048 + h * 128:2048 + (h + 1) * 128]], axis=1) for h in hs]))


def build_lin(T, N):
    nc = bass.Bass("TRN2", target_bir_lowering=False)
    S = Sched(nc)
    C = Ctx(nc, S)
    x_d = C.din("x", [T, D])
    w_d = C.din("w", [D, N])
    b_d = C.din("b", [N])
    id_d = C.din("identd", [128, 128])
    o_d = C.dout("yT", [N, T])
    ident = S.sbuf("ident", [128, 128], F32)
    S.dma("sp", ident.t[:], id_d.ap(), writes=[ident])
    bcol = S.sbuf("bcol", [128, N // 128], F32)
    S.dma("sp", bcol.t[:], bass.AP(b_d, 0, [[1, 128], [128, N // 128]]), writes=[bcol], allow_slow_non_contiguous=True)
    stg = S.sbuf("stg", [128, 8, 512], F32)
    wb = S.sbuf("wb", [128, 8, N], BF16)
    for j in range(N // 512):
        S.dma("sp", stg.t[:], w_d.ap()[:, j * 512:(j + 1) * 512].rearrange("(k p) n -> p k n", p=128), writes=[stg])
        S.op("pool", lambda e, j=j: e.tensor_copy(wb.t[:, :, j * 512:(j + 1) * 512], stg.t[:]), reads=[stg], writes=[wb])
    xin = [S.sbuf("xin%d" % i, [128, D], F32) for i in range(2)]
    xT = S.sbuf("xT", [128, 8, 512], BF16)
    osb = [S.sbuf("osb%d" % i, [128, 512], F32) for i in range(2)]
    pst = S.psum("pst", [128, 512], F32)
    pso = [S.psum("pso%d" % i, [128, 512], F32) for i in range(2)]
    for blk in range(T // 512):
        t0 = blk * 512
        for j in range(4):
            xi = xin[j % 2]
            S.dma("sp", xi.t[:], x_d.ap()[t0 + j * 128:t0 + (j + 1) * 128, :], writes=[xi])
            for g in range(0, 8, 4):
                for jj in range(4):
                    S.op("pe", lambda e, g=g, jj=jj, xi=xi: e.transpose(pst.t[:, jj * 128:(jj + 1) * 128], xi.t[:, (g + jj) * 128:(g + jj + 1) * 128], ident.t[:]),
                         reads=[xi, ident], writes=[pst])
                S.op("act", lambda e, g=g, j=j: e.activation(xT.t[:, g:g + 4, j * 128:(j + 1) * 128], pst.t[:].rearrange("p (a b) -> p a b", a=4), AF.Copy),
                     reads=[pst], writes=[xT])
        for n_ in range(N // 128):
            pp, ob = pso[n_ % 2], osb[n_ % 2]
            for kc in range(8):
                S.op("pe", lambda e, kc=kc, n_=n_, pp=pp: e.matmul(pp.t[:], wb.t[:, kc, n_ * 128:(n_ + 1) * 128], xT.t[:, kc, :], start=(kc == 0), stop=(kc == 7)),
                     reads=[wb, xT], writes=[pp])
            S.op("act", lambda e, n_=n_, pp=pp, ob=ob: e.activation(ob.t[:], pp.t[:], AF.Identity, bias=bcol.t[:, n_:n_ + 1], scale=1.0),
                 reads=[pp, bcol], writes=[ob])
            S.dma("sp", o_d.ap()[n_ * 128:(n_ + 1) * 128, t0:t0 + 512], ob.t[:], reads=[ob], writes=[S.dbuf("o%d_%d" % (blk, n_))], final=True)
    S.emit()
    return nc


def build_proj(T, pre_ln):
    nc = bass.Bass("TRN2", target_bir_lowering=False)
    S = Sched(nc)
    C = Ctx(nc, S)
    h_d = C.din("hT", [D, T])
    w_d = C.din("w", [D, D])
    b_d = C.din("b", [D])
    cg_d = C.din("cgb", [2, D])
    o_d = C.dout("h", [T, D])
    bbc = S.sbuf("bbc", [128, D], F32)
    S.dma("sp", bbc.t[:], bcast_rows(b_d, 0, D), writes=[bbc])
    cgb = S.sbuf("cgbs", [128, 16], F32)
    S.dma("sp", cgb.t[:], bass.AP(cg_d, 0, [[1, 128], [128, 16]]), writes=[cgb], allow_slow_non_contiguous=True)
    onesf = S.sbuf("onesf", [128, 128], F32)
    S.op("dve", lambda e: e.memset(onesf.t[:], 1.0 / D), writes=[onesf])
    stg = S.sbuf("stg", [128, 8, 512], F32)
    wb = S.sbuf("wb", [128, 8, D], BF16)
    for j in range(2):
        S.dma("sp", stg.t[:], w_d.ap()[:, j * 512:(j + 1) * 512].rearrange("(k p) n -> p k n", p=128), writes=[stg])
        S.op("pool", lambda e, j=j: e.tensor_copy(wb.t[:, :, j * 512:(j + 1) * 512], stg.t[:]), reads=[stg], writes=[wb])
    hf = [S.sbuf("hf%d" % i, [128, 8, 512], F32) for i in range(2)]
    hb = S.sbuf("hb", [128, 8, 512], BF16)
    sq = S.sbuf("sq", [128, 512], F32)
    st = S.sbuf("st", [128, 3, 512], F32)
    osb = [S.sbuf("osb%d" % i, [128, 512], F32) for i in range(2)]
    psa = S.psum("psa", [128, 512], F32)
    psb = S.psum("psb", [128, 512], F32)
    pso = [S.psum("pso%d" % i, [128, 512], F32) for i in range(2)]
    for blk in range(T // 512):
        t0 = blk * 512
        h = hf[blk % 2]
        S.dma("sp", h.t[:], h_d.ap()[:, t0:t0 + 512].rearrange("(k p) t -> p k t", p=128), writes=[h])
        if pre_ln:
            for kc in range(8):
                S.op("pe", lambda e, kc=kc, h=h: e.matmul(psa.t[:], onesf.t[:], h.t[:, kc, :], start=(kc == 0), stop=(kc == 7)), reads=[onesf, h], writes=[psa])
            for kc in range(8):
                S.op("dve", lambda e, kc=kc, h=h: e.tensor_tensor(sq.t[:], h.t[:, kc, :], h.t[:, kc, :], ALU.mult), reads=[h], writes=[sq])
                S.op("pe", lambda e, kc=kc: e.matmul(psb.t[:], onesf.t[:], sq.t[:], start=(kc == 0), stop=(kc == 7)), reads=[onesf, sq], writes=[psb])
            S.op("dve", lambda e: e.tensor_copy(st.t[:, 0, :], psa.t[:]), reads=[psa], writes=[st])
            S.op("dve", lambda e: e.tensor_tensor(st.t[:, 1, :], st.t[:, 0, :], st.t[:, 0, :], ALU.mult), reads=[st], writes=[st])
            S.op("dve", lambda e: e.tensor_tensor(st.t[:, 1, :], psb.t[:], st.t[:, 1, :], ALU.subtract), reads=[st, psb], writes=[st])
            S.op("dve", lambda e: e.tensor_scalar(st.t[:, 1, :], st.t[:, 1, :], LN_EPS, None, ALU.add), reads=[st], writes=[st])
            S.op("act", lambda e: e.activation(st.t[:, 1, :], st.t[:, 1, :], AF.Sqrt), reads=[st], writes=[st])
            S.op("dve", lambda e: e.reciprocal(st.t[:, 1, :], st.t[:, 1, :]), reads=[st], writes=[st])
            for kc in range(8):
                S.op("dve", lambda e, kc=kc, h=h: e.tensor_tensor(h.t[:, kc, :], h.t[:, kc, :], st.t[:, 0, :], ALU.subtract), reads=[h, st], writes=[h])
                S.op("dve", lambda e, kc=kc, h=h: e.tensor_tensor(h.t[:, kc, :], h.t[:, kc, :], st.t[:, 1, :], ALU.mult), reads=[h, st], writes=[h])
                S.op("dve", lambda e, kc=kc, h=h: e.tensor_scalar(h.t[:, kc, :], h.t[:, kc, :], cgb.t[:, kc:kc + 1], cgb.t[:, 8 + kc:9 + kc], ALU.mult, ALU.add),
                     reads=[h, cgb], writes=[h])
                S.op("act", lambda e, kc=kc, h=h: e.activation(hb.t[:, kc, :], h.t[:, kc, :], AF.Silu), reads=[h], writes=[hb])
        else:
            S.op("pool", lambda e, h=h: e.tensor_copy(hb.t[:], h.t[:]), reads=[h], writes=[hb])
        for tt in range(4):
            for hf_ in range(2):
                pp, ob = pso[hf_], osb[hf_]
                for kc in range(8):
                    S.op("pe", lambda e, kc=kc, tt=tt, hf_=hf_, pp=pp: e.matmul(pp.t[:], hb.t[:, kc, tt * 128:(tt + 1) * 128], wb.t[:, kc, hf_ * 512:(hf_ + 1) * 512],
                                                                            start=(kc == 0), stop=(kc == 7)), reads=[hb, wb], writes=[pp])
                S.op("dve", lambda e, hf_=hf_, pp=pp, ob=ob: e.tensor_tensor(ob.t[:], pp.t[:], bbc.t[:, hf_ * 512:(hf_ + 1) * 512], ALU.add), reads=[pp, bbc], writes=[ob])
                S.dma("sp", o_d.ap()[t0 + tt * 128:t0 + (tt + 1) * 128, hf_ * 512:(hf_ + 1) * 512], ob.t[:], reads=[ob],
                      writes=[S.dbuf("o%d_%d_%d" % (blk, tt, hf_))], final=True)
    S.emit()
    return nc


def build_dwconf(NCH):
    nc = bass.Bass("TRN2", target_bir_lowering=False)
    S = Sched(nc)
    C = Ctx(nc, S)
    u_d = C.din("u", [NCH, 2, 128, 2078])
    w_d = C.din("wdw", [128, 32])
    o_d = C.dout("y", [NCH, 128, 2048])
    w = S.sbuf("w_sb", [128, 32], F32)
    S.dma("sp", w.t[:], w_d.ap(), writes=[w])
    a = [S.sbuf("a%d" % i, [128, 2078], F32) for i in range(2)]
    g = [S.sbuf("g%d" % i, [128, 2078], F32) for i in range(2)]
    acc = [S.sbuf("acc%d" % i, [128, 2048], F32) for i in range(2)]
    for ch in range(NCH):
        ab, gb, ac = a[ch % 2], g[ch % 2], acc[ch % 2]
        S.dma("sp", ab.t[:], u_d.ap()[ch, 0], writes=[ab])
        S.dma("sp", gb.t[:], u_d.ap()[ch, 1], writes=[gb])
        S.op("act", lambda e, gb=gb: e.activation(gb.t[:], gb.t[:], AF.Sigmoid), reads=[gb], writes=[gb])
        S.op("dve", lambda e, ab=ab, gb=gb: e.tensor_tensor(ab.t[:], ab.t[:], gb.t[:], ALU.mult), reads=[ab, gb], writes=[ab])
        S.op("dve", lambda e, ab=ab, ac=ac: e.tensor_scalar(ac.t[:], ab.t[:, 0:2048], w.t[:, 0:1], w.t[:, 31:32], ALU.mult, ALU.add), reads=[ab, w], writes=[ac])
        for j in range(1, 31):
            S.op("dve", lambda e, j=j, ab=ab, ac=ac: e.scalar_tensor_tensor(ac.t[:], ab.t[:, j:j + 2048], w.t[:, j:j + 1], ac.t[:], ALU.mult, ALU.add),
                 reads=[ab, w, ac], writes=[ac])
        S.dma("sp", o_d.ap()[ch], ac.t[:], reads=[ac], writes=[S.dbuf("y%d" % ch)], final=True)
    S.emit()
    return nc


def build_hyena(LP, LS, NSEQ, GS):
    nc = bass.Bass("TRN2", target_bir_lowering=False)
    S = Sched(nc)
    C = Ctx(nc, S)
    TT = LP + NSEQ * LS
    u_d = C.din("u", [3, 128, TT])
    wsh_d = C.din("wsh", [128, 12])
    fb_d = C.din("fbias", [128, 2])
    ztp_d = C.din("zt_p", [33, LP])
    zts_d = C.din("zt_s", [33, LS])
    w1_d = C.din("fw1", [33, 64])
    w2_d = C.din("fw2", [64, 64])
    w3_d = C.din("fw3", [64, 64])
    fv_d = C.din("fvec", [64, 4])
    wo_d = C.din("fwout", [64, 4 * 128])
    dcp_d = C.din("dec_p", [128, LP])
    dcs_d = C.din("dec_s", [128, LS])
    z_d = C.dout("z", [128, TT])
    uc_d = nc.dram_tensor("uc", [3, 128, TT], F32, kind="Internal")
    hp_d = nc.dram_tensor("hfil_p", [4, 128, LP], F32, kind="Internal")
    hs_d = nc.dram_tensor("hfil_s", [4, 128, LS], F32, kind="Internal")

    def cload(name, shape, src):
        b = S.sbuf(name + "_sb", shape, F32)
        S.dma("sp", b.t[:], src, writes=[b])
        return b
    wsh = cload("wsh", [128, 12], wsh_d.ap())
    fbs = cload("fbias", [128, 2], fb_d.ap())
    w1 = cload("fw1", [33, 64], w1_d.ap())
    w2 = cload("fw2", [64, 64], w2_d.ap())
    w3 = cload("fw3", [64, 64], w3_d.ap())
    fv = cload("fvec", [64, 4], fv_d.ap())
    wo = cload("fwout", [64, 512], wo_d.ap())
    fsc = S.sbuf("fsc", [64, 4], F32)
    S.op("dve", lambda e: e.tensor_scalar(fsc.t[:, 0:1], fv.t[:, 0:1], 1.0 / 3.0, None, ALU.mult), reads=[fv], writes=[fsc])
    S.op("dve", lambda e: e.tensor_scalar(fsc.t[:, 1:4], fv.t[:, 1:4], fsc.t[:, 0:1], None, ALU.mult), reads=[fv, fsc], writes=[fsc])

    sb = [S.sbuf("scb%d" % i, [128, 2050], F32) for i in range(1)] * 2
    so = [S.sbuf("sco%d" % i, [128, 2048], F32) for i in range(1)] * 2
    seqs = [(0, LP)] + [(LP + i * LS, LS) for i in range(NSEQ)]
    k = 0
    for sl in range(3):
        for (s0, L) in seqs:
            for ci in range(L // 2048):
                b, o = sb[k % 2], so[k % 2]
                k += 1
                t0 = s0 + ci * 2048
                lo = 0 if ci == 0 else -1
                hi = 2048 if ci == L // 2048 - 1 else 2049
                if lo == 0:
                    S.op("pool", lambda e, b=b: e.memset(b.t[:, 0:1], 0.0), writes=[b])
                if hi == 2048:
                    S.op("pool", lambda e, b=b: e.memset(b.t[:, 2049:2050], 0.0), writes=[b])
                S.dma("sp", b.t[:, 1 + lo:1 + hi], u_d.ap()[sl, :, t0 + lo:t0 + hi], reads=[b], writes=[b])
                S.op("dve", lambda e, b=b, o=o, sl=sl: e.tensor_scalar(o.t[:], b.t[:, 0:2048], wsh.t[:, 3 * sl:3 * sl + 1], wsh.t[:, 9 + sl:10 + sl], ALU.mult, ALU.add),
                     reads=[b, wsh], writes=[o])
                for j in (1, 2):
                    S.op("dve", lambda e, b=b, o=o, sl=sl, j=j: e.scalar_tensor_tensor(o.t[:], b.t[:, j:j + 2048], wsh.t[:, 3 * sl + j:3 * sl + j + 1], o.t[:], ALU.mult, ALU.add),
                         reads=[b, wsh, o], writes=[o])
                S.dma("sp", uc_d.ap()[sl, :, t0:t0 + 2048], o.t[:], reads=[o], writes=[S.dbuf("uc%d_%d" % (sl, t0))])

    zt = [S.sbuf("zt%d" % i, [33, 512], F32) for i in range(2)]
    hm = [S.sbuf("hm%d" % i, [64, 512], F32) for i in range(3)]
    tq = S.sbuf("tq", [64, 512], F32)
    dc = [S.sbuf("dc%d" % i, [128, 512], F32) for i in range(2)]
    fo = [S.sbuf("fo%d" % i, [128, 512], F32) for i in range(2)]
    psm = [S.psum("psm%d" % i, [128, 512], F32) for i in range(2)]
    psf = [S.psum("psf%d" % i, [128, 512], F32) for i in range(2)]
    k = 0
    for (L, zsrc, dsrc, hdst, tag) in ((LP, ztp_d, dcp_d, hp_d, "p"), (LS, zts_d, dcs_d, hs_d, "s")):
        for blk in range(L // 512):
            z, d = zt[blk % 2], dc[blk % 2]
            S.dma("sp", z.t[:], zsrc.ap()[:, blk * 512:(blk + 1) * 512], writes=[z])
            S.dma("sp", d.t[:], dsrc.ap()[:, blk * 512:(blk + 1) * 512], writes=[d])
            src, srcb, K_ = z.t[:], z, 33
            for li, wl in enumerate((w1, w2, w3)):
                pm, h = psm[li % 2], hm[li]
                S.op("pe", lambda e, pm=pm, wl=wl, src=src, K_=K_: e.matmul(pm.t[0:64, :], wl.t[0:K_, :], src, start=True, stop=True), reads=[wl, srcb], writes=[pm])
                S.op("act", lambda e, pm=pm, h=h, li=li: e.activation(h.t[:], pm.t[0:64, :], AF.Sin, bias=fsc.t[:, 1 + li:2 + li], scale=fsc.t[:, 0:1]), reads=[pm, fsc], writes=[h])
                S.op("dve", lambda e, h=h: e.tensor_tensor(tq.t[:], h.t[:], h.t[:], ALU.mult), reads=[h], writes=[tq])
                S.op("dve", lambda e: e.tensor_scalar(tq.t[:], tq.t[:], -4.0, 3.0, ALU.mult, ALU.add), reads=[tq], writes=[tq])
                S.op("dve", lambda e, h=h: e.tensor_tensor(h.t[:], h.t[:], tq.t[:], ALU.mult), reads=[h, tq], writes=[h])
                src, srcb, K_ = h.t[:], h, 64
            for nd in range(4):
                pf, f = psf[nd % 2], fo[k % 2]
                k += 1
                S.op("pe", lambda e, pf=pf, nd=nd: e.matmul(pf.t[:], wo.t[:, nd * 128:(nd + 1) * 128], hm[2].t[:], start=True, stop=True), reads=[wo, hm[2]], writes=[pf])
                S.op("dve", lambda e, pf=pf, f=f, d=d: e.tensor_tensor(f.t[:], pf.t[:], d.t[:], ALU.mult), reads=[pf, d], writes=[f])
                S.dma("sp", hdst.ap()[nd, :, blk * 512:(blk + 1) * 512], f.t[:], reads=[f], writes=[S.dbuf("hf%s" % tag)])

    vbuf = S.sbuf("vbuf", [128, LP], F32)
    obuf = S.sbuf("obuf", [128, LP], F32)
    tf = [S.sbuf("tf%d" % i, [128, 2048], F32) for i in range(1)] * 2
    tb = [S.sbuf("tb%d" % i, [128, 2048], F32) for i in range(1)] * 2
    gt = [S.sbuf("gt%d" % i, [128, 2048], F32) for i in range(1)] * 2
    c0 = S.sbuf("c0", [128, 1], F32)
    groups = [(0, LP, 1, hp_d, "p")] + [(LP + gi * GS * LS, LS, GS, hs_d, "s") for gi in range(NSEQ // GS)]
    k = 0
    for (s0, L, ns, hsrc, tag) in groups:
        V = vbuf.t[:, 0:ns * L].rearrange("p (s l) -> p s l", s=ns)
        O = obuf.t[:, 0:ns * L].rearrange("p (s l) -> p s l", s=ns)
        S.dma("sp", vbuf.t[:, 0:ns * L], uc_d.ap()[2, :, s0:s0 + ns * L],
              reads=[S.dbuf("uc2_%d" % t) for t in range(s0, s0 + ns * L, 2048)], writes=[vbuf])
        for n in range(2):
            for cb in range(L // 2048):
                f, b = tf[k % 2], tb[k % 2]
                k += 1
                S.dma("sp", f.t[:], hsrc.ap()[2 * n, :, cb * 2048:(cb + 1) * 2048], reads=[S.dbuf("hf%s" % tag)], writes=[f])
                S.dma("sp", b.t[:], hsrc.ap()[2 * n + 1, :, cb * 2048:(cb + 1) * 2048], reads=[S.dbuf("hf%s" % tag)], writes=[b])
                for tl in range(2048):
                    tau = cb * 2048 + tl
                    if tau == 0:
                        S.op("dve", lambda e, f=f, b=b: e.tensor_tensor(c0.t[:], f.t[:, 0:1], b.t[:, 0:1], ALU.add), reads=[f, b], writes=[c0])
                        S.op("dve", lambda e, V=V, O=O: e.tensor_scalar(O, V, c0.t[:, 0:1], None, ALU.mult), reads=[vbuf, c0], writes=[obuf])
                        continue
                    S.op("dve", lambda e, V=V, O=O, f=f, tl=tl, tau=tau, L=L: e.scalar_tensor_tensor(O[:, :, tau:L], V[:, :, 0:L - tau], f.t[:, tl:tl + 1], O[:, :, tau:L], ALU.mult, ALU.add),
                         reads=[vbuf, f, obuf], writes=[obuf])
                    S.op("dve", lambda e, V=V, O=O, b=b, tl=tl, tau=tau, L=L: e.scalar_tensor_tensor(O[:, :, 0:L - tau], V[:, :, tau:L], b.t[:, tl:tl + 1], O[:, :, 0:L - tau], ALU.mult, ALU.add),
                         reads=[vbuf, b, obuf], writes=[obuf])
            for sq_ in range(ns):
                for cb in range(L // 2048):
                    g = gt[k % 2]
                    k += 1
                    tok = s0 + sq_ * L + cb * 2048
                    S.dma("sp", g.t[:], uc_d.ap()[n, :, tok:tok + 2048], reads=[S.dbuf("uc%d_%d" % (n, tok))], writes=[g])
                    vs = vbuf.t[:, sq_ * L + cb * 2048:sq_ * L + (cb + 1) * 2048]
                    os_ = obuf.t[:, sq_ * L + cb * 2048:sq_ * L + (cb + 1) * 2048]
                    S.op("dve", lambda e, vs=vs, os_=os_, n=n: e.scalar_tensor_tensor(os_, vs, fbs.t[:, n:n + 1], os_, ALU.mult, ALU.add), reads=[vbuf, obuf, fbs], writes=[obuf])
                    S.op("dve", lambda e, vs=vs, os_=os_, g=g: e.tensor_tensor(vs, os_, g.t[:], ALU.mult), reads=[obuf, g], writes=[vbuf])
        S.dma("sp", z_d.ap()[:, s0:s0 + ns * L], vbuf.t[:, 0:ns * L], reads=[vbuf], writes=[S.dbuf("z%d" % s0)], final=True)
    S.emit()
    return nc


NCORE = 8
LP_ = 16384
LS_ = 2048
NSAMP = 32
CH = LP_ // NCORE
SPC = NSAMP // NCORE
TPC = CH + SPC * LS_
_PROGS = {}


def _prog(key, fn):
    if key not in _PROGS:
        _PROGS[key] = fn()
    return _PROGS[key]


def _run(nc, in_maps):
    res = run_bass_kernel_spmd(nc, in_maps, core_ids=list(range(NCORE)))
    return res.results


def _c(a):
    return np.ascontiguousarray(a, dtype=np.float32)


def shard_tok(xp, xs):
    return [_c(np.concatenate([xp[c * CH:(c + 1) * CH]] + [xs[SPC * c + k] for k in range(SPC)], axis=0)) for c in range(NCORE)]


def unshard_tok(parts):
    xp = np.concatenate([p[:CH] for p in parts], axis=0)
    xs = np.stack([parts[c][CH + k * LS_:CH + (k + 1) * LS_] for c in range(NCORE) for k in range(SPC)])
    return xp, xs


def shard_feat(fp, fs):
    return [_c(np.concatenate([fp[:, c * CH:(c + 1) * CH]] + [fs[SPC * c + k] for k in range(SPC)], axis=1)) for c in range(NCORE)]


def unshard_feat(parts):
    fp = np.concatenate([p[:, :CH] for p in parts], axis=1)
    fs = np.stack([parts[c][:, CH + k * LS_:CH + (k + 1) * LS_] for c in range(NCORE) for k in range(SPC)])
    return fp, fs


def hyena_pos(L):
    t = np.linspace(0.0, 1.0, L, dtype=np.float32)[:, None]
    w = (np.float32(2.0 * np.pi) * np.arange(L, dtype=np.float32)[:, None] / np.float32(L)).astype(np.float32)
    bands = np.linspace(1e-4, 15, 16, dtype=np.float32)[None, :]
    fw = (bands * w).astype(np.float32)
    z = np.concatenate([t, np.cos(fw), -np.sin(fw)], axis=-1).astype(np.float32)
    min_decay = np.log(1e-2) / 1.5
    max_decay = np.log(1e-2) / 0.3
    deltas = np.abs(np.linspace(min_decay, max_decay, D, dtype=np.float32))
    decay = np.exp(-t * deltas[None, :]).astype(np.float32)
    return _c(z.T), decay


def kernel(x_prompt, x_sample, p_prompt, p_sample,
           attn_w_qkv, attn_w_o, attn_lam_q1, attn_lam_k1, attn_lam_q2, attn_lam_k2, attn_subln_g,
           conv_w_pw1, conv_b_pw1, conv_w_dw, conv_b_dw, conv_ln_g, conv_ln_b, conv_w_pw2, conv_b_pw2,
           hy_w_in, hy_b_in, hy_w_short, hy_b_short, hy_f_w1, hy_f_b1, hy_f_w2, hy_f_b2, hy_f_w3, hy_f_b3,
           hy_f_freq, hy_f_wout, hy_f_bias, hy_w_out, hy_b_out,
           ln1_g, ln1_b, ln2_g, ln2_b,
           moe_w_group, moe_b_group, moe_w_expert, moe_b_expert, moe_w_gate, moe_w_up, moe_w_down,
           ple_w_up, ple_w_gate):
    A = lambda a: np.asarray(a, dtype=np.float32)
    xp = A(x_prompt)[0]
    xs = A(x_sample)
    p_prompt, p_sample = A(p_prompt), A(p_sample)
    ident = np.eye(128, dtype=np.float32)
    zeros_d = np.zeros(D, np.float32)
    for i in range(4):
        kind, j = i % 3, i // 3
        if kind == 0:
            li = 0.8 - 0.6 * float(np.exp(-0.3 * i))
            nc = _prog("attn", lambda: build_attn(LP_, LS_, SPC))
            lamv = _c(np.stack([A(attn_lam_q1)[j], A(attn_lam_k1)[j], A(attn_lam_q2)[j], A(attn_lam_k2)[j]]))
            wq = A(attn_w_qkv)[j]
            maps = [dict(attn_consts(c, li), xp=_c(xp), xs=_c(xs[SPC * c:SPC * (c + 1)]), w_heads=attn_heads(wq, c),
                         lamv=lamv, subg=_c(A(attn_subln_g)[j])) for c in range(NCORE)]
            res = _run(nc, maps)
            fp = np.concatenate([res[c]["hp"] for c in range(NCORE)], axis=0)
            fs = np.concatenate([res[c]["hs"] for c in range(NCORE)], axis=0)
            w_h, b_h, pre_ln, cgb = A(attn_w_o)[j], zeros_d, False, np.zeros((2, D), np.float32)
        elif kind == 1:
            nc = _prog("lin2048", lambda: build_lin(TPC, 2048))
            maps = [dict(x=xt, w=_c(A(conv_w_pw1)[j]), b=_c(A(conv_b_pw1)[j]), identd=ident) for xt in shard_tok(xp, xs)]
            up, us = unshard_feat([r["yT"] for r in _run(nc, maps)])
            seqs = [up] + [us[s] for s in range(NSAMP)]
            chunks = []
            for sq in seqs:
                Ls = sq.shape[1]
                pad = np.zeros((2048, Ls + 30), np.float32)
                pad[:, 15:15 + Ls] = sq
                for ci in range(Ls // 2048):
                    chunks.append(pad[:, ci * 2048:ci * 2048 + 2078])
            nch = len(chunks)
            nc = _prog("dwconf", lambda: build_dwconf(nch))
            wdw = A(conv_w_dw)[j]
            bdw = A(conv_b_dw)[j]
            maps = []
            for c in range(NCORE):
                u = np.stack([np.stack([ck[c * 128:(c + 1) * 128], ck[1024 + c * 128:1024 + (c + 1) * 128]]) for ck in chunks])
                maps.append(dict(u=_c(u), wdw=_c(np.concatenate([wdw[:, c * 128:(c + 1) * 128].T, bdw[c * 128:(c + 1) * 128, None]], axis=1))))
            res = _run(nc, maps)
            y = np.concatenate([res[c]["y"] for c in range(NCORE)], axis=1)
            fp = np.concatenate([y[ci] for ci in range(LP_ // 2048)], axis=1)
            fs = y[LP_ // 2048:]
            w_h, b_h, pre_ln = A(conv_w_pw2)[j], A(conv_b_pw2)[j], True
            cgb = _c(np.stack([A(conv_ln_g)[j], A(conv_ln_b)[j]]))
        else:
            nc = _prog("lin3072", lambda: build_lin(TPC, 3072))
            maps = [dict(x=xt, w=_c(A(hy_w_in)[j]), b=_c(A(hy_b_in)[j]), identd=ident) for xt in shard_tok(xp, xs)]
            up, us = unshard_feat([r["yT"] for r in _run(nc, maps)])
            U = np.concatenate([up] + [us[s] for s in range(NSAMP)], axis=1)
            nc = _prog("hyena", lambda: build_hyena(LP_, LS_, NSAMP, 8))
            ztp, decp = hyena_pos(LP_)
            zts, decs = hyena_pos(LS_)
            wsh, bsh = A(hy_w_short)[j], A(hy_b_short)[j]
            fwo = A(hy_f_wout)[j]
            fvec = _c(np.stack([A(hy_f_freq)[j], A(hy_f_b1)[j], A(hy_f_b2)[j], A(hy_f_b3)[j]], axis=1))
            maps = []
            for c in range(NCORE):
                sl = [slice(k * 1024 + c * 128, k * 1024 + (c + 1) * 128) for k in range(3)]
                maps.append(dict(
                    u=_c(np.stack([U[s] for s in sl])),
                    wsh=_c(np.concatenate([wsh[:, s].T for s in sl] + [bsh[s][:, None] for s in sl], axis=1)),
                    fbias=_c(A(hy_f_bias)[j][:, c * 128:(c + 1) * 128].T),
                    zt_p=ztp, zt_s=zts, fw1=_c(A(hy_f_w1)[j]), fw2=_c(A(hy_f_w2)[j]), fw3=_c(A(hy_f_w3)[j]), fvec=fvec,
                    fwout=_c(np.concatenate([fwo[:, nd * 1024 + c * 128:nd * 1024 + (c + 1) * 128] for nd in range(4)], axis=1)),
                    dec_p=_c(decp[:, c * 128:(c + 1) * 128].T), dec_s=_c(decs[:, c * 128:(c + 1) * 128].T)))
            res = _run(nc, maps)
            Z = np.concatenate([res[c]["z"] for c in range(NCORE)], axis=0)
            fp = Z[:, :LP_]
            fs = np.stack([Z[:, LP_ + s * LS_:LP_ + (s + 1) * LS_] for s in range(NSAMP)])
            w_h, b_h, pre_ln, cgb = A(hy_w_out)[j], A(hy_b_out)[j], False, np.zeros((2, D), np.float32)
        nc = _prog("proj%d" % pre_ln, lambda: build_proj(TPC, pre_ln))
        maps = [dict(hT=ht, w=_c(w_h), b=_c(b_h), cgb=cgb) for ht in shard_feat(fp, fs)]
        hsh = [r["h"] for r in _run(nc, maps)]
        nc = _prog("tok", lambda: build_tok(TPC))
        common = tok_weights(A(ln1_g)[i], A(ln1_b)[i], A(ln2_g)[i], A(ln2_b)[i], A(moe_w_group)[i], A(moe_b_group)[i],
                             A(moe_w_expert)[i], A(moe_b_expert)[i], A(moe_w_gate)[i], A(moe_w_up)[i], A(moe_w_down)[i],
                             A(ple_w_up)[i], A(ple_w_gate)[i])
        xsh = shard_tok(xp, xs)
        psh = shard_tok(p_prompt[i, 0], p_sample[i])
        maps = [dict(common, x=xsh[c], h=_c(hsh[c]), p=psh[c]) for c in range(NCORE)]
        xp, xs = unshard_tok([r["xo"] for r in _run(nc, maps)])
    return (np.ascontiguousarray(xp[None].astype(np.float32)), np.ascontiguousarray(xs.astype(np.float32)))
```

```python
from contextlib import ExitStack
import numpy as np
import ml_dtypes
import concourse.bass as bass
import concourse.mybir as mybir
from concourse.bass_utils import run_bass_kernel_spmd

F32 = mybir.dt.float32
BF16 = mybir.dt.bfloat16
AF = mybir.ActivationFunctionType
ALU = mybir.AluOpType
AX = mybir.AxisListType

ENGS = ("pe", "act", "dve", "pool", "sp")


class Buf:
    __slots__ = ("name", "t", "w", "r", "sems", "cnt", "ps")

    def __init__(self, name, t=None, ps=False):
        self.name = name
        self.t = t
        self.ps = ps
        self.w = None
        self.r = []
        self.sems = None
        self.cnt = 0


class Op:
    __slots__ = ("eng", "fn", "deps", "sig", "is_dma", "key", "final", "sem", "val", "idx")


class Sched:
    SEM_LIMIT = 24000

    def __init__(self, nc):
        self.nc = nc
        self.stack = ExitStack()
        self.ops = []
        self._dbufs = {}

    def sbuf(self, name, shape, dtype):
        return Buf(name, self.stack.enter_context(self.nc.sbuf_tensor(name, list(shape), dtype)))

    def psum(self, name, shape, dtype):
        return Buf(name, self.stack.enter_context(self.nc.psum_tensor(name, list(shape), dtype)), ps=True)

    def dbuf(self, name):
        b = self._dbufs.get(name)
        if b is None:
            b = self._dbufs[name] = Buf(name)
        return b

    def _add(self, eng, fn, reads, writes, is_dma, key, final):
        op = Op()
        op.eng = eng
        op.fn = fn
        op.sig = final or is_dma
        op.is_dma = is_dma
        op.key = key
        op.final = final
        op.sem = None
        op.val = 0
        op.idx = len(self.ops)
        deps = {}
        for b in reads:
            if b.w is not None:
                deps[b.w.idx] = b.w
            if b.ps:
                for r in b.r:
                    if r.eng != eng:
                        deps[r.idx] = r
        for b in writes:
            if b.w is not None:
                deps[b.w.idx] = b.w
            for r in b.r:
                deps[r.idx] = r
        deps.pop(op.idx, None)
        for b in reads:
            b.r.append(op)
        for b in writes:
            b.w = op
            b.r = []
        dl = []
        for d in deps.values():
            if d.eng == "pe" and eng == "pe" and not is_dma and not d.is_dma:
                continue
            d.sig = True
            dl.append(d)
        op.deps = dl
        self.ops.append(op)
        return op

    def op(self, eng, fn, reads=(), writes=()):
        return self._add(eng, fn, reads, writes, False, None, False)

    def dma(self, eng, out, in_, reads=(), writes=(), final=False, key=None, **kw):
        if key is None:
            for b in list(writes) + list(reads):
                if b.t is not None:
                    key = b
                    break
            else:
                key = (list(writes) + list(reads))[0]
        return self._add(eng, lambda e: e.dma_start(out=out, in_=in_, **kw), reads, writes, True, key, final)

    def emit(self):
        nc = self.nc
        stack = self.stack
        n_sem = [0]

        def newsem():
            n_sem[0] += 1
            return stack.enter_context(nc.semaphore("s%d" % n_sem[0]))

        eng_sems = {e: [] for e in ENGS}
        eng_cnt = {e: 0 for e in ENGS}
        lim = self.SEM_LIMIT
        for op in self.ops:
            if not op.sig:
                continue
            if op.is_dma:
                b = op.key
                if b.sems is None:
                    b.sems = []
                k = b.cnt
                b.cnt += 1
                per = lim // 16
                while len(b.sems) <= k // per:
                    b.sems.append(newsem())
                op.sem = b.sems[k // per]
                op.val = 16 * (k % per + 1)
            else:
                k = eng_cnt[op.eng]
                eng_cnt[op.eng] += 1
                sl = eng_sems[op.eng]
                while len(sl) <= k // lim:
                    sl.append(newsem())
                op.sem = sl[k // lim]
                op.val = k % lim + 1
        queues = {e: [] for e in ENGS}
        for op in self.ops:
            queues[op.eng].append(op)
        finals = [op for op in self.ops if op.final]
        self.n_sems = n_sem[0]

        def run_queue(e, ops, last):
            known = {}
            for op in ops:
                need = {}
                for d in op.deps:
                    sid = id(d.sem)
                    if known.get(sid, 0) >= d.val:
                        continue
                    cur = need.get(sid)
                    if cur is None or cur[1] < d.val:
                        need[sid] = (d.sem, d.val)
                for sid, (sem, val) in need.items():
                    e.wait_ge(sem, val)
                    known[sid] = val
                ins = op.fn(e)
                if op.sig:
                    ins.then_inc(op.sem, 16 if op.is_dma else 1)
            if last:
                for op in finals:
                    e.wait_ge(op.sem, op.val)

        with nc.Block() as block:
            @block.tensor
            def _(e):
                run_queue(e, queues["pe"], False)

            @block.scalar
            def _(e):
                run_queue(e, queues["act"], False)

            @block.vector
            def _(e):
                run_queue(e, queues["dve"], False)

            @block.gpsimd
            def _(e):
                run_queue(e, queues["pool"], False)

            @block.sync
            def _(e):
                run_queue(e, queues["sp"], True)
        stack.close()


D = 1024
ALPHA = 8 ** 0.25
LN_EPS = 1e-5
NEG = -1.0e30


def bcast_rows(dt, off, n, parts=128):
    return bass.AP(dt, off, [[0, parts], [1, n]])


class Ctx:
    def __init__(self, nc, S):
        self.nc = nc
        self.S = S
        self.uid = 0

    def din(self, name, shape, dtype=F32):
        return self.nc.dram_tensor(name, list(shape), dtype, kind="ExternalInput")

    def dout(self, name, shape, dtype=F32):
        return self.nc.dram_tensor(name, list(shape), dtype, kind="ExternalOutput")


def layer_norm_tile(S, src, dst, g_bc, b_bc, scr):
    st, mv, tmp = scr
    for j in range(2):
        S.op("dve", lambda e, j=j: e.bn_stats(st.t[:, j * 6:(j + 1) * 6], src.t[:, j * 512:(j + 1) * 512]),
             reads=[src], writes=[st])
    S.op("dve", lambda e: e.bn_aggr(mv.t[:, 0:2], st.t[:, 0:12]), reads=[st], writes=[mv])
    S.op("dve", lambda e: e.tensor_scalar(mv.t[:, 2:3], mv.t[:, 1:2], LN_EPS, None, ALU.add), reads=[mv], writes=[mv])
    S.op("act", lambda e: e.activation(mv.t[:, 3:4], mv.t[:, 2:3], AF.Sqrt), reads=[mv], writes=[mv])
    S.op("dve", lambda e: e.reciprocal(mv.t[:, 4:5], mv.t[:, 3:4]), reads=[mv], writes=[mv])
    S.op("dve", lambda e: e.tensor_scalar(tmp.t[:], src.t[:], mv.t[:, 0:1], mv.t[:, 4:5], ALU.subtract, ALU.mult),
         reads=[src, mv], writes=[tmp])
    S.op("dve", lambda e: e.tensor_tensor(tmp.t[:], tmp.t[:], g_bc.t[:], ALU.mult), reads=[tmp, g_bc], writes=[tmp])
    S.op("dve", lambda e: e.tensor_tensor(dst.t[:], tmp.t[:], b_bc.t[:], ALU.add), reads=[tmp, b_bc], writes=[dst])


def transpose_tile(S, src_ap_fn, src_buf, ident, pst, nk, evac):
    for g in range(0, nk, 4):
        n = min(4, nk - g)
        for j in range(n):
            S.op("pe", lambda e, g=g, j=j: e.transpose(pst.t[:, j * 128:(j + 1) * 128], src_ap_fn(g + j), ident.t[:]),
                 reads=[src_buf, ident], writes=[pst])
        evac(g, n)


def build_tok(T):
    nc = bass.Bass("TRN2", target_bir_lowering=False)
    S = Sched(nc)
    C = Ctx(nc, S)
    NB = T // 512
    x_d = C.din("x", [T, D])
    h_d = C.din("h", [T, D])
    p_d = C.din("p", [T, 256])
    vec_d = C.din("vecs", [4, D])
    wr_d = C.din("w_router", [D, 20])
    br_d = C.din("b_router", [20])
    wg_d = C.din("w_gate", [16, D, 512])
    wu_d = C.din("w_up", [16, D, 512])
    wd_d = C.din("w_down", [16, 512, D])
    pu_d = C.din("ple_up", [256, D])
    pg_d = C.din("ple_gate", [D, D])
    id_d = C.din("identd", [128, 128])
    o_d = C.dout("xo", [T, D])

    ident = S.sbuf("ident", [128, 128], F32)
    S.dma("sp", ident.t[:], id_d.ap(), writes=[ident])
    vb = []
    for i in range(4):
        b = S.sbuf("vec%d" % i, [128, D], F32)
        S.dma("sp", b.t[:], bcast_rows(vec_d, i * D, D), writes=[b])
        vb.append(b)
    wr = S.sbuf("wr", [128, 8, 20], F32)
    S.dma("sp", wr.t[:], wr_d.ap().rearrange("(k p) n -> p k n", p=128), writes=[wr])
    brb = S.sbuf("brb", [128, 20], F32)
    S.dma("sp", brb.t[:], bcast_rows(br_d, 0, 20), writes=[brb])
    stg = [S.sbuf("stg%d" % i, [128, 8, 512], F32) for i in range(2)]
    pgw = S.sbuf("pgw", [128, 8, D], BF16)
    puw = S.sbuf("puw", [128, 2, D], BF16)
    for hf in range(2):
        S.dma("sp", stg[hf].t[:], pg_d.ap()[:, hf * 512:(hf + 1) * 512].rearrange("(k p) n -> p k n", p=128), writes=[stg[hf]])
        S.op("pool", lambda e, hf=hf: e.tensor_copy(pgw.t[:, :, hf * 512:(hf + 1) * 512], stg[hf].t[:]), reads=[stg[hf]], writes=[pgw])
    for hf in range(2):
        S.dma("sp", stg[hf].t[:, 0:2, :], pu_d.ap()[:, hf * 512:(hf + 1) * 512].rearrange("(k p) n -> p k n", p=128), writes=[stg[hf]])
        S.op("pool", lambda e, hf=hf: e.tensor_copy(puw.t[:, :, hf * 512:(hf + 1) * 512], stg[hf].t[:, 0:2, :]), reads=[stg[hf]], writes=[puw])

    xin = [S.sbuf("xin%d" % i, [128, D], F32) for i in range(2)]
    hin = [S.sbuf("hin%d" % i, [128, D], F32) for i in range(2)]
    pin = [S.sbuf("pin%d" % i, [128, 256], F32) for i in range(2)]
    x1t = S.sbuf("x1t", [128, D], F32)
    lnscr = (S.sbuf("lnst", [128, 12], F32), S.sbuf("lnmv", [128, 8], F32), S.sbuf("lntmp", [128, D], F32))
    acc = S.sbuf("acc", [128, 4, D], F32)
    accb = [Buf("acc%d" % i) for i in range(4)]
    xT = S.sbuf("xT", [128, 8, 512], BF16)
    xTf = S.sbuf("xTf", [128, 8, 128], F32)
    pT = S.sbuf("pT", [128, 2, 512], BF16)
    pTf = S.sbuf("pTf", [128, 2, 128], F32)
    lg = S.sbuf("lg", [128, 20], F32)
    rt = S.sbuf("rt", [128, 96], F32)
    comb = S.sbuf("comb", [128, 4, 16], F32)
    wgb = [S.sbuf("wgb%d" % i, [128, 8, 512], BF16) for i in range(2)]
    wub = [S.sbuf("wub%d" % i, [128, 8, 512], BF16) for i in range(2)]
    wdb = [S.sbuf("wdb%d" % i, [128, 4, D], BF16) for i in range(2)]
    sg = [S.sbuf("sg%d" % i, [128, 512], F32) for i in range(2)]
    hid = [S.sbuf("hid%d" % i, [128, 4, 512], BF16) for i in range(2)]
    osb = [S.sbuf("osb%d" % i, [128, 512], F32) for i in range(2)]
    pst = S.psum("pst", [128, 512], F32)
    psr = S.psum("psr", [128, 512], F32)
    psg = [S.psum("psg%d" % i, [128, 512], F32) for i in range(2)]
    psu = [S.psum("psu%d" % i, [128, 512], F32) for i in range(2)]
    psd = [S.psum("psd%d" % i, [128, 512], F32) for i in range(2)]
    wcnt = [0]

    def load_w(dst, src_ap, nk):
        st = stg[wcnt[0] % 2]
        wcnt[0] += 1
        n = src_ap.shape[-1]
        if n == 512:
            S.dma("sp", st.t[:, 0:nk, :], src_ap.rearrange("(k p) n -> p k n", p=128), writes=[st])
            S.op("pool", lambda e: e.tensor_copy(dst.t[:, 0:nk, :], st.t[:, 0:nk, :]), reads=[st], writes=[dst])
        else:
            stv = st.t[:].rearrange("p a b -> p (a b)").rearrange("p (k n) -> p k n", k=nk)
            S.dma("sp", stv, src_ap.rearrange("(k p) n -> p k n", p=128), writes=[st])
            S.op("pool", lambda e: e.tensor_copy(dst.t[:, 0:nk, :], stv), reads=[st], writes=[dst])

    for blk in range(NB):
        t0 = blk * 512
        for tt in range(4):
            r0 = t0 + tt * 128
            xi, hi = xin[tt % 2], hin[tt % 2]
            S.dma("sp", xi.t[:], x_d.ap()[r0:r0 + 128, :], writes=[xi])
            S.dma("sp", hi.t[:], h_d.ap()[r0:r0 + 128, :], writes=[hi])
            S.op("dve", lambda e, xi=xi, hi=hi: e.scalar_tensor_tensor(xi.t[:], xi.t[:], ALPHA, hi.t[:], ALU.mult, ALU.add),
                 reads=[xi, hi], writes=[xi])
            layer_norm_tile(S, xi, x1t, vb[0], vb[1], lnscr)
            S.op("act", lambda e, tt=tt: e.mul(acc.t[:, tt, :], x1t.t[:], ALPHA), reads=[x1t], writes=[accb[tt]])

            def evac(g, n, tt=tt):
                S.op("act", lambda e: e.activation(xTf.t[:, g:g + n, :], pst.t[:, 0:n * 128].rearrange("p (a b) -> p a b", a=n), AF.Copy),
                     reads=[pst], writes=[xTf])
                S.op("pool", lambda e: e.tensor_copy(xT.t[:, g:g + n, tt * 128:(tt + 1) * 128], xTf.t[:, g:g + n, :]),
                     reads=[xTf], writes=[xT])
            transpose_tile(S, lambda k: x1t.t[:, k * 128:(k + 1) * 128], x1t, ident, pst, 8, evac)
            for kc in range(8):
                S.op("pe", lambda e, kc=kc: e.matmul(psr.t[:, 0:20], xTf.t[:, kc, :], wr.t[:, kc, :], start=(kc == 0), stop=(kc == 7)),
                     reads=[xTf, wr], writes=[psr])
            S.op("dve", lambda e: e.tensor_tensor(lg.t[:], psr.t[:, 0:20], brb.t[:], ALU.add), reads=[psr, brb], writes=[lg])
            R = rt.t
            dv = lambda fn, rd=(rt, lg), wrb=(rt,): S.op("dve", fn, reads=list(rd), writes=list(wrb))
            dv(lambda e: e.tensor_reduce(R[:, 0:1], lg.t[:, 0:4], AX.X, ALU.max))
            dv(lambda e: e.tensor_scalar(R[:, 4:8], lg.t[:, 0:4], R[:, 0:1], None, ALU.is_equal))
            dv(lambda e: e.tensor_scalar(R[:, 1:2], R[:, 0:1], -1.0, None, ALU.mult))
            S.op("act", lambda e: e.activation(R[:, 8:12], lg.t[:, 0:4], AF.Exp, bias=R[:, 1:2], scale=1.0),
                 reads=[rt, lg], writes=[rt])
            dv(lambda e: e.tensor_reduce(R[:, 2:3], R[:, 8:12], AX.X, ALU.add))
            dv(lambda e: e.reciprocal(R[:, 3:4], R[:, 2:3]))
            dv(lambda e: e.tensor_scalar(R[:, 12:16], R[:, 4:8], 1.0, -NEG, ALU.subtract, ALU.mult))
            for g in range(4):
                dv(lambda e, g=g: e.tensor_scalar(R[:, 16 + 4 * g:20 + 4 * g], lg.t[:, 4 + 4 * g:8 + 4 * g],
                                                  R[:, 12 + g:13 + g], None, ALU.add))
            dv(lambda e: e.max(R[:, 32:40], R[:, 16:32]))
            dv(lambda e: e.tensor_scalar(R[:, 40:56], R[:, 16:32], R[:, 32:33], None, ALU.is_equal))
            dv(lambda e: e.tensor_scalar(R[:, 56:72], R[:, 16:32], R[:, 33:34], None, ALU.is_equal))
            dv(lambda e: e.tensor_tensor(R[:, 72:73], R[:, 32:33], R[:, 33:34], ALU.subtract))
            S.op("act", lambda e: e.activation(R[:, 73:74], R[:, 72:73], AF.Sigmoid), reads=[rt], writes=[rt])
            dv(lambda e: e.tensor_tensor(R[:, 74:75], R[:, 73:74], R[:, 3:4], ALU.mult))
            dv(lambda e: e.tensor_tensor(R[:, 75:76], R[:, 3:4], R[:, 74:75], ALU.subtract))
            dv(lambda e: e.tensor_scalar(R[:, 76:92], R[:, 40:56], R[:, 74:75], None, ALU.mult))
            S.op("dve", lambda e, tt=tt: e.scalar_tensor_tensor(comb.t[:, tt, :], R[:, 56:72], R[:, 75:76], R[:, 76:92], ALU.mult, ALU.add),
                 reads=[rt], writes=[comb])
        for ex in range(16):
            wg, wu, wd = wgb[ex % 2], wub[ex % 2], wdb[ex % 2]
            load_w(wg, wg_d.ap()[ex], 8)
            load_w(wu, wu_d.ap()[ex], 8)
            load_w(wd, wd_d.ap()[ex], 4)
            hb = hid[ex % 2]
            for fc in range(4):
                pg_, pu_, sgb = psg[fc % 2], psu[fc % 2], sg[fc % 2]
                for kc in range(8):
                    S.op("pe", lambda e, kc=kc, fc=fc, pg_=pg_, wg=wg: e.matmul(pg_.t[:], wg.t[:, kc, fc * 128:(fc + 1) * 128], xT.t[:, kc, :],
                                                                               start=(kc == 0), stop=(kc == 7)), reads=[wg, xT], writes=[pg_])
                for kc in range(8):
                    S.op("pe", lambda e, kc=kc, fc=fc, pu_=pu_, wu=wu: e.matmul(pu_.t[:], wu.t[:, kc, fc * 128:(fc + 1) * 128], xT.t[:, kc, :],
                                                                               start=(kc == 0), stop=(kc == 7)), reads=[wu, xT], writes=[pu_])
                S.op("act", lambda e, pg_=pg_, sgb=sgb: e.activation(sgb.t[:], pg_.t[:], AF.Silu), reads=[pg_], writes=[sgb])
                S.op("dve", lambda e, fc=fc, pu_=pu_, sgb=sgb, hb=hb: e.tensor_tensor(hb.t[:, fc, :], sgb.t[:], pu_.t[:], ALU.mult),
                     reads=[sgb, pu_], writes=[hb])
            for tt in range(4):
                for hf in range(2):
                    pd = psd[(tt * 2 + hf) % 2]
                    for fc in range(4):
                        S.op("pe", lambda e, fc=fc, tt=tt, hf=hf, pd=pd, hb=hb, wd=wd: e.matmul(
                            pd.t[:], hb.t[:, fc, tt * 128:(tt + 1) * 128], wd.t[:, fc, hf * 512:(hf + 1) * 512],
                            start=(fc == 0), stop=(fc == 3)), reads=[hb, wd], writes=[pd])
                    S.op("dve", lambda e, tt=tt, hf=hf, pd=pd, ex=ex: e.scalar_tensor_tensor(
                        acc.t[:, tt, hf * 512:(hf + 1) * 512], pd.t[:], comb.t[:, tt, ex:ex + 1], acc.t[:, tt, hf * 512:(hf + 1) * 512],
                        ALU.mult, ALU.add), reads=[pd, comb, accb[tt]], writes=[accb[tt]])
        for tt in range(4):
            r0 = t0 + tt * 128
            a_view = Buf("accv")
            a_view.t = acc.t[:, tt, :]
            src = accb[tt]
            src.t = acc.t[:, tt, :]
            layer_norm_tile(S, src, src, vb[2], vb[3], lnscr)
            pi = pin[tt % 2]
            S.dma("sp", pi.t[:], p_d.ap()[r0:r0 + 128, :], writes=[pi])

            def evac2(g, n, tt=tt):
                S.op("act", lambda e: e.activation(xTf.t[:, g:g + n, :], pst.t[:, 0:n * 128].rearrange("p (a b) -> p a b", a=n), AF.Copy),
                     reads=[pst], writes=[xTf])
                S.op("pool", lambda e: e.tensor_copy(xT.t[:, g:g + n, tt * 128:(tt + 1) * 128], xTf.t[:, g:g + n, :]),
                     reads=[xTf], writes=[xT])
            transpose_tile(S, lambda k, tt=tt: acc.t[:, tt, k * 128:(k + 1) * 128], src, ident, pst, 8, evac2)

            def evac3(g, n, tt=tt):
                S.op("act", lambda e: e.activation(pTf.t[:, g:g + n, :], pst.t[:, 0:n * 128].rearrange("p (a b) -> p a b", a=n), AF.Copy),
                     reads=[pst], writes=[pTf])
                S.op("pool", lambda e: e.tensor_copy(pT.t[:, g:g + n, tt * 128:(tt + 1) * 128], pTf.t[:, g:g + n, :]),
                     reads=[pTf], writes=[pT])
            transpose_tile(S, lambda k, pi=pi: pi.t[:, k * 128:(k + 1) * 128], pi, ident, pst, 2, evac3)
            for hf in range(2):
                pg_, pu_, sgb, ob = psg[hf], psu[hf], sg[hf], osb[hf]
                for kc in range(8):
                    S.op("pe", lambda e, kc=kc, tt=tt, hf=hf, pg_=pg_: e.matmul(pg_.t[:], xT.t[:, kc, tt * 128:(tt + 1) * 128],
                                                                              pgw.t[:, kc, hf * 512:(hf + 1) * 512], start=(kc == 0), stop=(kc == 7)),
                         reads=[xT, pgw], writes=[pg_])
                for kc in range(2):
                    S.op("pe", lambda e, kc=kc, tt=tt, hf=hf, pu_=pu_: e.matmul(pu_.t[:], pT.t[:, kc, tt * 128:(tt + 1) * 128],
                                                                              puw.t[:, kc, hf * 512:(hf + 1) * 512], start=(kc == 0), stop=(kc == 1)),
                         reads=[pT, puw], writes=[pu_])
                S.op("act", lambda e, pg_=pg_, sgb=sgb: e.activation(sgb.t[:], pg_.t[:], AF.Sigmoid), reads=[pg_], writes=[sgb])
                S.op("dve", lambda e, pu_=pu_, sgb=sgb: e.tensor_tensor(sgb.t[:], sgb.t[:], pu_.t[:], ALU.mult), reads=[sgb, pu_], writes=[sgb])
                S.op("dve", lambda e, tt=tt, hf=hf, sgb=sgb, ob=ob: e.tensor_tensor(ob.t[:], sgb.t[:], acc.t[:, tt, hf * 512:(hf + 1) * 512], ALU.add),
                     reads=[sgb, src], writes=[ob])
                S.dma("sp", o_d.ap()[r0:r0 + 128, hf * 512:(hf + 1) * 512], ob.t[:], reads=[ob], writes=[S.dbuf("xo%d_%d" % (r0, hf))], final=True)
    S.emit()
    return nc


def tok_weights(ln1_g, ln1_b, ln2_g, ln2_b, w_group, b_group, w_expert, b_expert, w_gate, w_up, w_down, ple_up, ple_gate):
    return dict(
        vecs=np.ascontiguousarray(np.stack([ln1_g, ln1_b, ln2_g, ln2_b]).astype(np.float32)),
        w_router=np.ascontiguousarray(np.concatenate([w_group, w_expert], axis=1)),
        b_router=np.ascontiguousarray(np.concatenate([b_group, b_expert])),
        w_gate=np.ascontiguousarray(w_gate), w_up=np.ascontiguousarray(w_up), w_down=np.ascontiguousarray(w_down),
        ple_up=np.ascontiguousarray(ple_up), ple_gate=np.ascontiguousarray(ple_gate),
        identd=np.eye(128, dtype=np.float32))


def build_attn(LP, LS, NS):
    nc = bass.Bass("TRN2", target_bir_lowering=False)
    S = Sched(nc)
    C = Ctx(nc, S)
    xp_d = C.din("xp", [LP, D])
    xs_d = C.din("xs", [NS, LS, D])
    wh_d = C.din("w_heads", [9, D, 384])
    lam_d = C.din("lamv", [4, 64])
    g_d = C.din("subg", [128])
    li_d = C.din("laminit", [2])
    rl_d = C.din("rl", [128, 512])
    td_d = C.din("td", [128, 4 * 512])
    hsc_d = C.din("hsc", [128, 9 * 130])
    id_d = C.din("identd", [128, 128])
    op_d = C.dout("hp", [128, LP])
    os_d = C.dout("hs", [NS, D, LS])
    LM = max(LP, LS)

    def cload(name, shape, src):
        b = S.sbuf(name + "_sb", shape, F32)
        S.dma("sp", b.t[:], src, writes=[b])
        return b
    ident = cload("ident", [128, 128], id_d.ap())
    rl = cload("rl", [128, 512], rl_d.ap())
    td = cload("td", [128, 2048], td_d.ap())
    hsc = cload("hsc", [128, 9 * 130], hsc_d.ap())
    lamv = cload("lamv", [128, 256], bcast_rows(lam_d, 0, 256))
    lin = cload("lin", [128, 2], bcast_rows(li_d, 0, 2))
    gcol = S.sbuf("gcol", [128, 1], F32)
    S.dma("sp", gcol.t[:], bass.AP(g_d, 0, [[1, 128], [1, 1]]), writes=[gcol])
    onesb = S.sbuf("onesb", [128, 128], BF16)
    onesf = S.sbuf("onesf", [128, 128], F32)
    S.op("dve", lambda e: e.memset(onesb.t[:], 1.0), writes=[onesb])
    S.op("dve", lambda e: e.memset(onesf.t[:], 1.0 / 128.0), writes=[onesf])
    lw = S.sbuf("lw", [128, 136], F32)
    S.op("dve", lambda e: e.tensor_tensor(lw.t[:, 0:64], lamv.t[:, 0:64], lamv.t[:, 64:128], ALU.mult), reads=[lamv], writes=[lw])
    S.op("dve", lambda e: e.tensor_tensor(lw.t[:, 64:128], lamv.t[:, 128:192], lamv.t[:, 192:256], ALU.mult), reads=[lamv, lw], writes=[lw])
    S.op("dve", lambda e: e.tensor_reduce(lw.t[:, 128:129], lw.t[:, 0:64], AX.X, ALU.add), reads=[lw], writes=[lw])
    S.op("dve", lambda e: e.tensor_reduce(lw.t[:, 129:130], lw.t[:, 64:128], AX.X, ALU.add), reads=[lw], writes=[lw])
    S.op("act", lambda e: e.activation(lw.t[:, 130:132], lw.t[:, 128:130], AF.Exp), reads=[lw], writes=[lw])
    S.op("dve", lambda e: e.tensor_tensor(lw.t[:, 132:133], lw.t[:, 131:132], lw.t[:, 130:131], ALU.subtract), reads=[lw], writes=[lw])
    S.op("dve", lambda e: e.tensor_tensor(lw.t[:, 133:134], lw.t[:, 132:133], lin.t[:, 0:1], ALU.subtract), reads=[lw, lin], writes=[lw])
    S.op("dve", lambda e: e.tensor_tensor(lw.t[:, 134:135], gcol.t[:], lin.t[:, 1:2], ALU.mult), reads=[lw, lin, gcol], writes=[lw])
    nlam = lw.t[:, 133:134]
    gsc = lw.t[:, 134:135]

    stg = S.sbuf("stg", [128, 8, 384], F32)
    wh = S.sbuf("wh", [128, 8, 384], BF16)
    xin = [S.sbuf("xin%d" % i, [128, D], F32) for i in range(2)]
    xT = S.sbuf("xT", [128, 8, LS], BF16)
    qT = S.sbuf("qT", [128, LM], BF16)
    kT = S.sbuf("kT", [128, LM], BF16)
    vv = S.sbuf("vv", [128, LM // 128, 128], BF16)
    tmp = [S.sbuf("tmp%d" % i, [128, 512], F32) for i in range(4)]
    Eb = [S.sbuf("E%d" % i, [128, 512], BF16) for i in range(4)]
    oc = [S.sbuf("oc%d" % i, [128, 512], F32) for i in range(2)]
    rs = S.sbuf("rs", [128, 512], F32)
    ob = [S.sbuf("ob%d" % i, [128, 512], F32) for i in range(2)]
    sq = S.sbuf("sq", [128, 512], F32)
    pst = S.psum("pst", [128, 512], F32)
    psp = [S.psum("psp%d" % i, [128, 512], F32) for i in range(2)]
    pss = [S.psum("pss%d" % i, [128, 512], F32) for i in range(2)]
    pso = S.psum("pso", [128, 512], F32)
    psn = S.psum("psn", [128, 512], F32)
    psm = S.psum("psm", [128, 512], F32)
    cnt = [0]

    def load_heads(hs):
        S.dma("sp", stg.t[:], wh_d.ap()[hs].rearrange("(k p) n -> p k n", p=128), writes=[stg])
        S.op("pool", lambda e: e.tensor_copy(wh.t[:], stg.t[:]), reads=[stg], writes=[wh])

    def transpose_block(src_rows_ap, c0):
        for j in range(4):
            xi = xin[j % 2]
            S.dma("sp", xi.t[:], src_rows_ap[j * 128:(j + 1) * 128, :], writes=[xi])
            for g in range(0, 8, 4):
                for jj in range(4):
                    S.op("pe", lambda e, g=g, jj=jj, xi=xi: e.transpose(pst.t[:, jj * 128:(jj + 1) * 128], xi.t[:, (g + jj) * 128:(g + jj + 1) * 128], ident.t[:]),
                         reads=[xi, ident], writes=[pst])
                S.op("act", lambda e, g=g, j=j: e.activation(xT.t[:, g:g + 4, c0 + j * 128:c0 + (j + 1) * 128],
                                                            pst.t[:].rearrange("p (a b) -> p a b", a=4), AF.Copy), reads=[pst], writes=[xT])

    def project(c0, t0):
        for which, dst in ((0, qT), (1, kT)):
            pp = psp[which]
            for kc in range(8):
                S.op("pe", lambda e, kc=kc, which=which, pp=pp: e.matmul(pp.t[:], wh.t[:, kc, which * 128:(which + 1) * 128], xT.t[:, kc, c0:c0 + 512],
                                                                       start=(kc == 0), stop=(kc == 7)), reads=[wh, xT], writes=[pp])
            S.op("act", lambda e, pp=pp, dst=dst: e.activation(dst.t[:, t0:t0 + 512], pp.t[:], AF.Copy), reads=[pp], writes=[dst])
        for j in range(4):
            for kc in range(8):
                S.op("pe", lambda e, kc=kc, j=j: e.matmul(pst.t[:, j * 128:(j + 1) * 128], xT.t[:, kc, c0 + j * 128:c0 + (j + 1) * 128], wh.t[:, kc, 256:384],
                                                        start=(kc == 0), stop=(kc == 7)), reads=[wh, xT], writes=[pst])
        S.op("act", lambda e: e.activation(vv.t[:, t0 // 128:t0 // 128 + 4, :], pst.t[:].rearrange("p (a b) -> p a b", a=4), AF.Copy),
             reads=[pst], writes=[vv])

    def attend(L, hs, out_ap_fn, out_name):
        nk = L // 128
        H = hsc.t
        hb = hs * 130
        for qi in range(L // 512):
            for c in range(2):
                pend = []

                def pv(kt, E):
                    S.op("pe", lambda e, kt=kt, E=E: e.matmul(pso.t[:], vv.t[:, kt, :], E.t[:], start=(kt == 0), stop=(kt == nk - 1)), reads=[vv, E], writes=[pso])
                    S.op("pe", lambda e, kt=kt, E=E: e.matmul(psn.t[:], onesb.t[:], E.t[:], start=(kt == 0), stop=(kt == nk - 1)), reads=[onesb, E], writes=[psn])
                for kt in range(nk):
                    dl = kt - 4 * qi
                    i = cnt[0] % 4
                    cnt[0] += 1
                    ps_, tm, E = (pss + psp)[i], tmp[i], Eb[i]
                    S.op("pe", lambda e, c=c, kt=kt, qi=qi, ps_=ps_: e.matmul(ps_.t[:], kT.t[c * 64:(c + 1) * 64, kt * 128:(kt + 1) * 128],
                                                                            qT.t[c * 64:(c + 1) * 64, qi * 512:(qi + 1) * 512], start=True, stop=True),
                         reads=[kT, qT], writes=[ps_])
                    if 0 <= dl <= 3:
                        tab, sc, bc = td.t[:, dl * 512:(dl + 1) * 512], H[:, hb:hb + 1], H[:, hb + 2:hb + 3]
                    elif dl < 0:
                        tab, sc, bc = rl.t[:], H[:, hb:hb + 1], H[:, hb + 2 - dl:hb + 3 - dl]
                    else:
                        tab, sc, bc = rl.t[:], H[:, hb + 1:hb + 2], H[:, hb + 2 + dl:hb + 3 + dl]
                    S.op("dve", lambda e, tab=tab, sc=sc, ps_=ps_, tm=tm: e.scalar_tensor_tensor(tm.t[:], tab, sc, ps_.t[:], ALU.mult, ALU.add),
                         reads=[td, rl, hsc, ps_], writes=[tm])
                    S.op("act", lambda e, tm=tm, E=E, bc=bc: e.activation(E.t[:], tm.t[:], AF.Exp, bias=bc, scale=0.125), reads=[tm, hsc], writes=[E])
                    pend.append((kt, E))
                    if len(pend) > 2:
                        pv(*pend.pop(0))
                while pend:
                    pv(*pend.pop(0))
                S.op("dve", lambda e: e.reciprocal(rs.t[:], psn.t[:]), reads=[psn], writes=[rs])
                S.op("dve", lambda e, c=c: e.tensor_tensor(oc[c].t[:], pso.t[:], rs.t[:], ALU.mult), reads=[pso, rs], writes=[oc[c]])
            o = ob[qi % 2]
            S.op("dve", lambda e, o=o: e.scalar_tensor_tensor(o.t[:], oc[1].t[:], nlam, oc[0].t[:], ALU.mult, ALU.add), reads=[oc[0], oc[1], lw], writes=[o])
            S.op("dve", lambda e, o=o: e.tensor_tensor(sq.t[:], o.t[:], o.t[:], ALU.mult), reads=[o], writes=[sq])
            S.op("pe", lambda e: e.matmul(psm.t[:], onesf.t[:], sq.t[:], start=True, stop=True), reads=[onesf, sq], writes=[psm])
            S.op("dve", lambda e: e.tensor_scalar(sq.t[:], psm.t[:], LN_EPS, None, ALU.add), reads=[psm], writes=[sq])
            S.op("act", lambda e: e.activation(sq.t[:], sq.t[:], AF.Sqrt), reads=[sq], writes=[sq])
            S.op("dve", lambda e: e.reciprocal(sq.t[:], sq.t[:]), reads=[sq], writes=[sq])
            S.op("dve", lambda e, o=o: e.tensor_tensor(o.t[:], o.t[:], sq.t[:], ALU.mult), reads=[o, sq], writes=[o])
            S.op("dve", lambda e, o=o: e.tensor_scalar(o.t[:], o.t[:], gsc, None, ALU.mult), reads=[o, lw], writes=[o])
            S.dma("sp", out_ap_fn(qi), o.t[:], reads=[o], writes=[S.dbuf("%s_%d" % (out_name, qi))], final=True)

    load_heads(8)
    for blk in range(LP // 512):
        transpose_block(xp_d.ap()[blk * 512:(blk + 1) * 512, :], 0)
        project(0, blk * 512)
    attend(LP, 8, lambda qi: op_d.ap()[:, qi * 512:(qi + 1) * 512], "hp")
    for s in range(NS):
        for blk in range(LS // 512):
            transpose_block(xs_d.ap()[s, blk * 512:(blk + 1) * 512, :], blk * 512)
        for h in range(8):
            load_heads(h)
            for blk in range(LS // 512):
                project(blk * 512, blk * 512)
            attend(LS, h, lambda qi, s=s, h=h: os_d.ap()[s, h * 128:(h + 1) * 128, qi * 512:(qi + 1) * 512], "hs%d_%d" % (s, h))
    S.emit()
    return nc


def attn_consts(core, lambda_init):
    p = np.arange(128, dtype=np.float32)[:, None]
    f = np.arange(512, dtype=np.float32)[None, :]
    rl = (f - p).astype(np.float32)
    td = np.stack([np.abs(f - p - 128.0 * j) for j in range(4)], axis=1).reshape(128, 2048).astype(np.float32)
    slopes = 2.0 ** (-8.0 * (np.arange(8, dtype=np.float64) + 1.0) / 8)
    hsc = np.zeros((9, 130), np.float64)
    for s in range(9):
        m = slopes[s] if s < 8 else slopes[core]
        hsc[s, 0] = -8.0 * m
        hsc[s, 1] = 8.0 * m
        hsc[s, 2:] = -m * 128.0 * np.arange(128)
    hsc = np.broadcast_to(hsc.reshape(1, -1), (128, 9 * 130)).astype(np.float32)
    return dict(rl=rl, td=td, hsc=np.ascontiguousarray(hsc), identd=np.eye(128, dtype=np.float32),
                laminit=np.array([lambda_init, 1.0 - lambda_init], np.float32))


def attn_heads(w_qkv, core):
    hs = list(range(8)) + [core]
    return np.ascontiguousarray(np.stack([
        np.concatenate([w_qkv[:, h * 128:(h + 1) * 128], w_qkv[:, 1024 + h * 128:1024 + (h + 1) * 128],
                        w_qkv[:, 2048 + h * 128:2048 + (h + 1) * 128]], axis=1) for h in hs]))


def attn_heads(w_qkv, core):
    hs = list(range(8)) + [core]
    return np.ascontiguousarray(np.stack([
        np.concatenate([w_qkv[:, h * 128:(h + 1) * 128], w_qkv[:, 1024 + h * 128:1024 + (h + 1) * 128],
                        w_qkv[:, 2048 + h * 128:2048 + (h + 1) * 128]], axis=1) for h in hs]))


def build_lin(T, N):
    nc = bass.Bass("TRN2", target_bir_lowering=False)
    S = Sched(nc)
    C = Ctx(nc, S)
    x_d = C.din("x", [T, D])
    w_d = C.din("w", [D, N])
    b_d = C.din("b", [N])
    id_d = C.din("identd", [128, 128])
    o_d = C.dout("yT", [N, T])
    ident = S.sbuf("ident", [128, 128], F32)
    S.dma("sp", ident.t[:], id_d.ap(), writes=[ident])
    bcol = S.sbuf("bcol", [128, N // 128], F32)
    S.dma("sp", bcol.t[:], bass.AP(b_d, 0, [[1, 128], [128, N // 128]]), writes=[bcol], allow_slow_non_contiguous=True)
    stg = S.sbuf("stg", [128, 8, 512], F32)
    wb = S.sbuf("wb", [128, 8, N], BF16)
    for j in range(N // 512):
        S.dma("sp", stg.t[:], w_d.ap()[:, j * 512:(j + 1) * 512].rearrange("(k p) n -> p k n", p=128), writes=[stg])
        S.op("pool", lambda e, j=j: e.tensor_copy(wb.t[:, :, j * 512:(j + 1) * 512], stg.t[:]), reads=[stg], writes=[wb])
    xin = [S.sbuf("xin%d" % i, [128, D], F32) for i in range(2)]
    xT = S.sbuf("xT", [128, 8, 512], BF16)
    osb = [S.sbuf("osb%d" % i, [128, 512], F32) for i in range(2)]
    pst = S.psum("pst", [128, 512], F32)
    pso = [S.psum("pso%d" % i, [128, 512], F32) for i in range(2)]
    for blk in range(T // 512):
        t0 = blk * 512
        for j in range(4):
            xi = xin[j % 2]
            S.dma("sp", xi.t[:], x_d.ap()[t0 + j * 128:t0 + (j + 1) * 128, :], writes=[xi])
            for g in range(0, 8, 4):
                for jj in range(4):
                    S.op("pe", lambda e, g=g, jj=jj, xi=xi: e.transpose(pst.t[:, jj * 128:(jj + 1) * 128], xi.t[:, (g + jj) * 128:(g + jj + 1) * 128], ident.t[:]),
                         reads=[xi, ident], writes=[pst])
                S.op("act", lambda e, g=g, j=j: e.activation(xT.t[:, g:g + 4, j * 128:(j + 1) * 128], pst.t[:].rearrange("p (a b) -> p a b", a=4), AF.Copy),
                     reads=[pst], writes=[xT])
        for n_ in range(N // 128):
            pp, ob = pso[n_ % 2], osb[n_ % 2]
            for kc in range(8):
                S.op("pe", lambda e, kc=kc, n_=n_, pp=pp: e.matmul(pp.t[:], wb.t[:, kc, n_ * 128:(n_ + 1) * 128], xT.t[:, kc, :], start=(kc == 0), stop=(kc == 7)),
                     reads=[wb, xT], writes=[pp])
            S.op("act", lambda e, n_=n_, pp=pp, ob=ob: e.activation(ob.t[:], pp.t[:], AF.Identity, bias=bcol.t[:, n_:n_ + 1], scale=1.0),
                 reads=[pp, bcol], writes=[ob])
            S.dma("sp", o_d.ap()[n_ * 128:(n_ + 1) * 128, t0:t0 + 512], ob.t[:], reads=[ob], writes=[S.dbuf("o%d_%d" % (blk, n_))], final=True)
    S.emit()
    return nc


def build_proj(T, pre_ln):
    nc = bass.Bass("TRN2", target_bir_lowering=False)
    S = Sched(nc)
    C = Ctx(nc, S)
    h_d = C.din("hT", [D, T])
    w_d = C.din("w", [D, D])
    b_d = C.din("b", [D])
    cg_d = C.din("cgb", [2, D])
    o_d = C.dout("h", [T, D])
    bbc = S.sbuf("bbc", [128, D], F32)
    S.dma("sp", bbc.t[:], bcast_rows(b_d, 0, D), writes=[bbc])
    cgb = S.sbuf("cgbs", [128, 16], F32)
    S.dma("sp", cgb.t[:], bass.AP(cg_d, 0, [[1, 128], [128, 16]]), writes=[cgb], allow_slow_non_contiguous=True)
    onesf = S.sbuf("onesf", [128, 128], F32)
    S.op("dve", lambda e: e.memset(onesf.t[:], 1.0 / D), writes=[onesf])
    stg = S.sbuf("stg", [128, 8, 512], F32)
    wb = S.sbuf("wb", [128, 8, D], BF16)
    for j in range(2):
        S.dma("sp", stg.t[:], w_d.ap()[:, j * 512:(j + 1) * 512].rearrange("(k p) n -> p k n", p=128), writes=[stg])
        S.op("pool", lambda e, j=j: e.tensor_copy(wb.t[:, :, j * 512:(j + 1) * 512], stg.t[:]), reads=[stg], writes=[wb])
    hf = [S.sbuf("hf%d" % i, [128, 8, 512], F32) for i in range(2)]
    hb = S.sbuf("hb", [128, 8, 512], BF16)
    sq = S.sbuf("sq", [128, 512], F32)
    st = S.sbuf("st", [128, 3, 512], F32)
    osb = [S.sbuf("osb%d" % i, [128, 512], F32) for i in range(2)]
    psa = S.psum("psa", [128, 512], F32)
    psb = S.psum("psb", [128, 512], F32)
    pso = [S.psum("pso%d" % i, [128, 512], F32) for i in range(2)]
    for blk in range(T // 512):
        t0 = blk * 512
        h = hf[blk % 2]
        S.dma("sp", h.t[:], h_d.ap()[:, t0:t0 + 512].rearrange("(k p) t -> p k t", p=128), writes=[h])
        if pre_ln:
            for kc in range(8):
                S.op("pe", lambda e, kc=kc, h=h: e.matmul(psa.t[:], onesf.t[:], h.t[:, kc, :], start=(kc == 0), stop=(kc == 7)), reads=[onesf, h], writes=[psa])
            for kc in range(8):
                S.op("dve", lambda e, kc=kc, h=h: e.tensor_tensor(sq.t[:], h.t[:, kc, :], h.t[:, kc, :], ALU.mult), reads=[h], writes=[sq])
                S.op("pe", lambda e, kc=kc: e.matmul(psb.t[:], onesf.t[:], sq.t[:], start=(kc == 0), stop=(kc == 7)), reads=[onesf, sq], writes=[psb])
            S.op("dve", lambda e: e.tensor_copy(st.t[:, 0, :], psa.t[:]), reads=[psa], writes=[st])
            S.op("dve", lambda e: e.tensor_tensor(st.t[:, 1, :], st.t[:, 0, :], st.t[:, 0, :], ALU.mult), reads=[st], writes=[st])
            S.op("dve", lambda e: e.tensor_tensor(st.t[:, 1, :], psb.t[:], st.t[:, 1, :], ALU.subtract), reads=[st, psb], writes=[st])
            S.op("dve", lambda e: e.tensor_scalar(st.t[:, 1, :], st.t[:, 1, :], LN_EPS, None, ALU.add), reads=[st], writes=[st])
            S.op("act", lambda e: e.activation(st.t[:, 1, :], st.t[:, 1, :], AF.Sqrt), reads=[st], writes=[st])
            S.op("dve", lambda e: e.reciprocal(st.t[:, 1, :], st.t[:, 1, :]), reads=[st], writes=[st])
            for kc in range(8):
                S.op("dve", lambda e, kc=kc, h=h: e.tensor_tensor(h.t[:, kc, :], h.t[:, kc, :], st.t[:, 0, :], ALU.subtract), reads=[h, st], writes=[h])
                S.op("dve", lambda e, kc=kc, h=h: e.tensor_tensor(h.t[:, kc, :], h.t[:, kc, :], st.t[:, 1, :], ALU.mult), reads=[h, st], writes=[h])
                S.op("dve", lambda e, kc=kc, h=h: e.tensor_scalar(h.t[:, kc, :], h.t[:, kc, :], cgb.t[:, kc:kc + 1], cgb.t[:, 8 + kc:9 + kc], ALU.mult, ALU.add),
                     reads=[h, cgb], writes=[h])
                S.op("act", lambda e, kc=kc, h=h: e.activation(hb.t[:, kc, :], h.t[:, kc, :], AF.Silu), reads=[h], writes=[hb])
        else:
            S.op("pool", lambda e, h=h: e.tensor_copy(hb.t[:], h.t[:]), reads=[h], writes=[hb])
        for tt in range(4):
            for hf_ in range(2):
                pp, ob = pso[hf_], osb[hf_]
                for kc in range(8):
                    S.op("pe", lambda e, kc=kc, tt=tt, hf_=hf_, pp=pp: e.matmul(pp.t[:], hb.t[:, kc, tt * 128:(tt + 1) * 128], wb.t[:, kc, hf_ * 512:(hf_ + 1) * 512],
                                                                            start=(kc == 0), stop=(kc == 7)), reads=[hb, wb], writes=[pp])
                S.op("dve", lambda e, hf_=hf_, pp=pp, ob=ob: e.tensor_tensor(ob.t[:], pp.t[:], bbc.t[:, hf_ * 512:(hf_ + 1) * 512], ALU.add), reads=[pp, bbc], writes=[ob])
                S.dma("sp", o_d.ap()[t0 + tt * 128:t0 + (tt + 1) * 128, hf_ * 512:(hf_ + 1) * 512], ob.t[:], reads=[ob],
                      writes=[S.dbuf("o%d_%d_%d" % (blk, tt, hf_))], final=True)
    S.emit()
    return nc


def build_dwconf(NCH):
    nc = bass.Bass("TRN2", target_bir_lowering=False)
    S = Sched(nc)
    C = Ctx(nc, S)
    u_d = C.din("u", [NCH, 2, 128, 2078])
    w_d = C.din("wdw", [128, 32])
    o_d = C.dout("y", [NCH, 128, 2048])
    w = S.sbuf("w_sb", [128, 32], F32)
    S.dma("sp", w.t[:], w_d.ap(), writes=[w])
    a = [S.sbuf("a%d" % i, [128, 2078], F32) for i in range(2)]
    g = [S.sbuf("g%d" % i, [128, 2078], F32) for i in range(2)]
    acc = [S.sbuf("acc%d" % i, [128, 2048], F32) for i in range(2)]
    for ch in range(NCH):
        ab, gb, ac = a[ch % 2], g[ch % 2], acc[ch % 2]
        S.dma("sp", ab.t[:], u_d.ap()[ch, 0], writes=[ab])
        S.dma("sp", gb.t[:], u_d.ap()[ch, 1], writes=[gb])
        S.op("act", lambda e, gb=gb: e.activation(gb.t[:], gb.t[:], AF.Sigmoid), reads=[gb], writes=[gb])
        S.op("dve", lambda e, ab=ab, gb=gb: e.tensor_tensor(ab.t[:], ab.t[:], gb.t[:], ALU.mult), reads=[ab, gb], writes=[ab])
        S.op("dve", lambda e, ab=ab, ac=ac: e.tensor_scalar(ac.t[:], ab.t[:, 0:2048], w.t[:, 0:1], w.t[:, 31:32], ALU.mult, ALU.add), reads=[ab, w], writes=[ac])
        for j in range(1, 31):
            S.op("dve", lambda e, j=j, ab=ab, ac=ac: e.scalar_tensor_tensor(ac.t[:], ab.t[:, j:j + 2048], w.t[:, j:j + 1], ac.t[:], ALU.mult, ALU.add),
                 reads=[ab, w, ac], writes=[ac])
        S.dma("sp", o_d.ap()[ch], ac.t[:], reads=[ac], writes=[S.dbuf("y%d" % ch)], final=True)
    S.emit()
    return nc


def build_hyena(LP, LS, NSEQ, GS):
    nc = bass.Bass("TRN2", target_bir_lowering=False)
    S = Sched(nc)
    C = Ctx(nc, S)
    TT = LP + NSEQ * LS
    u_d = C.din("u", [3, 128, TT])
    wsh_d = C.din("wsh", [128, 12])
    fb_d = C.din("fbias", [128, 2])
    ztp_d = C.din("zt_p", [33, LP])
    zts_d = C.din("zt_s", [33, LS])
    w1_d = C.din("fw1", [33, 64])
    w2_d = C.din("fw2", [64, 64])
    w3_d = C.din("fw3", [64, 64])
    fv_d = C.din("fvec", [64, 4])
    wo_d = C.din("fwout", [64, 4 * 128])
    dcp_d = C.din("dec_p", [128, LP])
    dcs_d = C.din("dec_s", [128, LS])
    z_d = C.dout("z", [128, TT])
    uc_d = nc.dram_tensor("uc", [3, 128, TT], F32, kind="Internal")
    hp_d = nc.dram_tensor("hfil_p", [4, 128, LP], F32, kind="Internal")
    hs_d = nc.dram_tensor("hfil_s", [4, 128, LS], F32, kind="Internal")

    def cload(name, shape, src):
        b = S.sbuf(name + "_sb", shape, F32)
        S.dma("sp", b.t[:], src, writes=[b])
        return b
    wsh = cload("wsh", [128, 12], wsh_d.ap())
    fbs = cload("fbias", [128, 2], fb_d.ap())
    w1 = cload("fw1", [33, 64], w1_d.ap())
    w2 = cload("fw2", [64, 64], w2_d.ap())
    w3 = cload("fw3", [64, 64], w3_d.ap())
    fv = cload("fvec", [64, 4], fv_d.ap())
    wo = cload("fwout", [64, 512], wo_d.ap())
    fsc = S.sbuf("fsc", [64, 4], F32)
    S.op("dve", lambda e: e.tensor_scalar(fsc.t[:, 0:1], fv.t[:, 0:1], 1.0 / 3.0, None, ALU.mult), reads=[fv], writes=[fsc])
    S.op("dve", lambda e: e.tensor_scalar(fsc.t[:, 1:4], fv.t[:, 1:4], fsc.t[:, 0:1], None, ALU.mult), reads=[fv, fsc], writes=[fsc])

    sb = [S.sbuf("scb%d" % i, [128, 2050], F32) for i in range(1)] * 2
    so = [S.sbuf("sco%d" % i, [128, 2048], F32) for i in range(1)] * 2
    seqs = [(0, LP)] + [(LP + i * LS, LS) for i in range(NSEQ)]
    k = 0
    for sl in range(3):
        for (s0, L) in seqs:
            for ci in range(L // 2048):
                b, o = sb[k % 2], so[k % 2]
                k += 1
                t0 = s0 + ci * 2048
                lo = 0 if ci == 0 else -1
                hi = 2048 if ci == L // 2048 - 1 else 2049
                if lo == 0:
                    S.op("pool", lambda e, b=b: e.memset(b.t[:, 0:1], 0.0), writes=[b])
                if hi == 2048:
                    S.op("pool", lambda e, b=b: e.memset(b.t[:, 2049:2050], 0.0), writes=[b])
                S.dma("sp", b.t[:, 1 + lo:1 + hi], u_d.ap()[sl, :, t0 + lo:t0 + hi], reads=[b], writes=[b])
                S.op("dve", lambda e, b=b, o=o, sl=sl: e.tensor_scalar(o.t[:], b.t[:, 0:2048], wsh.t[:, 3 * sl:3 * sl + 1], wsh.t[:, 9 + sl:10 + sl], ALU.mult, ALU.add),
                     reads=[b, wsh], writes=[o])
                for j in (1, 2):
                    S.op("dve", lambda e, b=b, o=o, sl=sl, j=j: e.scalar_tensor_tensor(o.t[:], b.t[:, j:j + 2048], wsh.t[:, 3 * sl + j:3 * sl + j + 1], o.t[:], ALU.mult, ALU.add),
                         reads=[b, wsh, o], writes=[o])
                S.dma("sp", uc_d.ap()[sl, :, t0:t0 + 2048], o.t[:], reads=[o], writes=[S.dbuf("uc%d_%d" % (sl, t0))])

    zt = [S.sbuf("zt%d" % i, [33, 512], F32) for i in range(2)]
    hm = [S.sbuf("hm%d" % i, [64, 512], F32) for i in range(3)]
    tq = S.sbuf("tq", [64, 512], F32)
    dc = [S.sbuf("dc%d" % i, [128, 512], F32) for i in range(2)]
    fo = [S.sbuf("fo%d" % i, [128, 512], F32) for i in range(2)]
    psm = [S.psum("psm%d" % i, [128, 512], F32) for i in range(2)]
    psf = [S.psum("psf%d" % i, [128, 512], F32) for i in range(2)]
    k = 0
    for (L, zsrc, dsrc, hdst, tag) in ((LP, ztp_d, dcp_d, hp_d, "p"), (LS, zts_d, dcs_d, hs_d, "s")):
        for blk in range(L // 512):
            z, d = zt[blk % 2], dc[blk % 2]
            S.dma("sp", z.t[:], zsrc.ap()[:, blk * 512:(blk + 1) * 512], writes=[z])
            S.dma("sp", d.t[:], dsrc.ap()[:, blk * 512:(blk + 1) * 512], writes=[d])
            src, srcb, K_ = z.t[:], z, 33
            for li, wl in enumerate((w1, w2, w3)):
                pm, h = psm[li % 2], hm[li]
                S.op("pe", lambda e, pm=pm, wl=wl, src=src, K_=K_: e.matmul(pm.t[0:64, :], wl.t[0:K_, :], src, start=True, stop=True), reads=[wl, srcb], writes=[pm])
                S.op("act", lambda e, pm=pm, h=h, li=li: e.activation(h.t[:], pm.t[0:64, :], AF.Sin, bias=fsc.t[:, 1 + li:2 + li], scale=fsc.t[:, 0:1]), reads=[pm, fsc], writes=[h])
                S.op("dve", lambda e, h=h: e.tensor_tensor(tq.t[:], h.t[:], h.t[:], ALU.mult), reads=[h], writes=[tq])
                S.op("dve", lambda e: e.tensor_scalar(tq.t[:], tq.t[:], -4.0, 3.0, ALU.mult, ALU.add), reads=[tq], writes=[tq])
                S.op("dve", lambda e, h=h: e.tensor_tensor(h.t[:], h.t[:], tq.t[:], ALU.mult), reads=[h, tq], writes=[h])
                src, srcb, K_ = h.t[:], h, 64
            for nd in range(4):
                pf, f = psf[nd % 2], fo[k % 2]
                k += 1
                S.op("pe", lambda e, pf=pf, nd=nd: e.matmul(pf.t[:], wo.t[:, nd * 128:(nd + 1) * 128], hm[2].t[:], start=True, stop=True), reads=[wo, hm[2]], writes=[pf])
                S.op("dve", lambda e, pf=pf, f=f, d=d: e.tensor_tensor(f.t[:], pf.t[:], d.t[:], ALU.mult), reads=[pf, d], writes=[f])
                S.dma("sp", hdst.ap()[nd, :, blk * 512:(blk + 1) * 512], f.t[:], reads=[f], writes=[S.dbuf("hf%s" % tag)])

    vbuf = S.sbuf("vbuf", [128, LP], F32)
    obuf = S.sbuf("obuf", [128, LP], F32)
    tf = [S.sbuf("tf%d" % i, [128, 2048], F32) for i in range(1)] * 2
    tb = [S.sbuf("tb%d" % i, [128, 2048], F32) for i in range(1)] * 2
    gt = [S.sbuf("gt%d" % i, [128, 2048], F32) for i in range(1)] * 2
    c0 = S.sbuf("c0", [128, 1], F32)
    groups = [(0, LP, 1, hp_d, "p")] + [(LP + gi * GS * LS, LS, GS, hs_d, "s") for gi in range(NSEQ // GS)]
    k = 0
    for (s0, L, ns, hsrc, tag) in groups:
        V = vbuf.t[:, 0:ns * L].rearrange("p (s l) -> p s l", s=ns)
        O = obuf.t[:, 0:ns * L].rearrange("p (s l) -> p s l", s=ns)
        S.dma("sp", vbuf.t[:, 0:ns * L], uc_d.ap()[2, :, s0:s0 + ns * L],
              reads=[S.dbuf("uc2_%d" % t) for t in range(s0, s0 + ns * L, 2048)], writes=[vbuf])
        for n in range(2):
            for cb in range(L // 2048):
                f, b = tf[k % 2], tb[k % 2]
                k += 1
                S.dma("sp", f.t[:], hsrc.ap()[2 * n, :, cb * 2048:(cb + 1) * 2048], reads=[S.dbuf("hf%s" % tag)], writes=[f])
                S.dma("sp", b.t[:], hsrc.ap()[2 * n + 1, :, cb * 2048:(cb + 1) * 2048], reads=[S.dbuf("hf%s" % tag)], writes=[b])
                for tl in range(2048):
                    tau = cb * 2048 + tl
                    if tau == 0:
                        S.op("dve", lambda e, f=f, b=b: e.tensor_tensor(c0.t[:], f.t[:, 0:1], b.t[:, 0:1], ALU.add), reads=[f, b], writes=[c0])
                        S.op("dve", lambda e, V=V, O=O: e.tensor_scalar(O, V, c0.t[:, 0:1], None, ALU.mult), reads=[vbuf, c0], writes=[obuf])
                        continue
                    S.op("dve", lambda e, V=V, O=O, f=f, tl=tl, tau=tau, L=L: e.scalar_tensor_tensor(O[:, :, tau:L], V[:, :, 0:L - tau], f.t[:, tl:tl + 1], O[:, :, tau:L], ALU.mult, ALU.add),
                         reads=[vbuf, f, obuf], writes=[obuf])
                    S.op("dve", lambda e, V=V, O=O, b=b, tl=tl, tau=tau, L=L: e.scalar_tensor_tensor(O[:, :, 0:L - tau], V[:, :, tau:L], b.t[:, tl:tl + 1], O[:, :, 0:L - tau], ALU.mult, ALU.add),
                         reads=[vbuf, b, obuf], writes=[obuf])
            for sq_ in range(ns):
                for cb in range(L // 2048):
                    g = gt[k % 2]
                    k += 1
                    tok = s0 + sq_ * L + cb * 2048
                    S.dma("sp", g.t[:], uc_d.ap()[n, :, tok:tok + 2048], reads=[S.dbuf("uc%d_%d" % (n, tok))], writes=[g])
                    vs = vbuf.t[:, sq_ * L + cb * 2048:sq_ * L + (cb + 1) * 2048]
                    os_ = obuf.t[:, sq_ * L + cb * 2048:sq_ * L + (cb + 1) * 2048]
                    S.op("dve", lambda e, vs=vs, os_=os_, n=n: e.scalar_tensor_tensor(os_, vs, fbs.t[:, n:n + 1], os_, ALU.mult, ALU.add), reads=[vbuf, obuf, fbs], writes=[obuf])
                    S.op("dve", lambda e, vs=vs, os_=os_, g=g: e.tensor_tensor(vs, os_, g.t[:], ALU.mult), reads=[obuf, g], writes=[vbuf])
        S.dma("sp", z_d.ap()[:, s0:s0 + ns * L], vbuf.t[:, 0:ns * L], reads=[vbuf], writes=[S.dbuf("z%d" % s0)], final=True)
    S.emit()
    return nc


NCORE = 8
LP_ = 16384
LS_ = 2048
NSAMP = 32
CH = LP_ // NCORE
SPC = NSAMP // NCORE
TPC = CH + SPC * LS_
_PROGS = {}


def _prog(key, fn):
    if key not in _PROGS:
        _PROGS[key] = fn()
    return _PROGS[key]


def _run(nc, in_maps):
    res = run_bass_kernel_spmd(nc, in_maps, core_ids=list(range(NCORE)))
    return res.results


def _c(a):
    return np.ascontiguousarray(a, dtype=np.float32)


def shard_tok(xp, xs):
    return [_c(np.concatenate([xp[c * CH:(c + 1) * CH]] + [xs[SPC * c + k] for k in range(SPC)], axis=0)) for c in range(NCORE)]


def unshard_tok(parts):
    xp = np.concatenate([p[:CH] for p in parts], axis=0)
    xs = np.stack([parts[c][CH + k * LS_:CH + (k + 1) * LS_] for c in range(NCORE) for k in range(SPC)])
    return xp, xs


def shard_feat(fp, fs):
    return [_c(np.concatenate([fp[:, c * CH:(c + 1) * CH]] + [fs[SPC * c + k] for k in range(SPC)], axis=1)) for c in range(NCORE)]


def unshard_feat(parts):
    fp = np.concatenate([p[:, :CH] for p in parts], axis=1)
    fs = np.stack([parts[c][:, CH + k * LS_:CH + (k + 1) * LS_] for c in range(NCORE) for k in range(SPC)])
    return fp, fs


def hyena_pos(L):
    t = np.linspace(0.0, 1.0, L, dtype=np.float32)[:, None]
    w = (np.float32(2.0 * np.pi) * np.arange(L, dtype=np.float32)[:, None] / np.float32(L)).astype(np.float32)
    bands = np.linspace(1e-4, 15, 16, dtype=np.float32)[None, :]
    fw = (bands * w).astype(np.float32)
    z = np.concatenate([t, np.cos(fw), -np.sin(fw)], axis=-1).astype(np.float32)
    min_decay = np.log(1e-2) / 1.5
    max_decay = np.log(1e-2) / 0.3
    deltas = np.abs(np.linspace(min_decay, max_decay, D, dtype=np.float32))
    decay = np.exp(-t * deltas[None, :]).astype(np.float32)
    return _c(z.T), decay


def kernel(x_prompt, x_sample, p_prompt, p_sample,
           attn_w_qkv, attn_w_o, attn_lam_q1, attn_lam_k1, attn_lam_q2, attn_lam_k2, attn_subln_g,
           conv_w_pw1, conv_b_pw1, conv_w_dw, conv_b_dw, conv_ln_g, conv_ln_b, conv_w_pw2, conv_b_pw2,
           hy_w_in, hy_b_in, hy_w_short, hy_b_short, hy_f_w1, hy_f_b1, hy_f_w2, hy_f_b2, hy_f_w3, hy_f_b3,
           hy_f_freq, hy_f_wout, hy_f_bias, hy_w_out, hy_b_out,
           ln1_g, ln1_b, ln2_g, ln2_b,
           moe_w_group, moe_b_group, moe_w_expert, moe_b_expert, moe_w_gate, moe_w_up, moe_w_down,
           ple_w_up, ple_w_gate):
    A = lambda a: np.asarray(a, dtype=np.float32)
    xp = A(x_prompt)[0]
    xs = A(x_sample)
    p_prompt, p_sample = A(p_prompt), A(p_sample)
    ident = np.eye(128, dtype=np.float32)
    zeros_d = np.zeros(D, np.float32)
    for i in range(4):
        kind, j = i % 3, i // 3
        if kind == 0:
            li = 0.8 - 0.6 * float(np.exp(-0.3 * i))
            nc = _prog("attn", lambda: build_attn(LP_, LS_, SPC))
            lamv = _c(np.stack([A(attn_lam_q1)[j], A(attn_lam_k1)[j], A(attn_lam_q2)[j], A(attn_lam_k2)[j]]))
            wq = A(attn_w_qkv)[j]
            maps = [dict(attn_consts(c, li), xp=_c(xp), xs=_c(xs[SPC * c:SPC * (c + 1)]), w_heads=attn_heads(wq, c),
                         lamv=lamv, subg=_c(A(attn_subln_g)[j])) for c in range(NCORE)]
            res = _run(nc, maps)
            fp = np.concatenate([res[c]["hp"] for c in range(NCORE)], axis=0)
            fs = np.concatenate([res[c]["hs"] for c in range(NCORE)], axis=0)
            w_h, b_h, pre_ln, cgb = A(attn_w_o)[j], zeros_d, False, np.zeros((2, D), np.float32)
        elif kind == 1:
            nc = _prog("lin2048", lambda: build_lin(TPC, 2048))
            maps = [dict(x=xt, w=_c(A(conv_w_pw1)[j]), b=_c(A(conv_b_pw1)[j]), identd=ident) for xt in shard_tok(xp, xs)]
            up, us = unshard_feat([r["yT"] for r in _run(nc, maps)])
            seqs = [up] + [us[s] for s in range(NSAMP)]
            chunks = []
            for sq in seqs:
                Ls = sq.shape[1]
                pad = np.zeros((2048, Ls + 30), np.float32)
                pad[:, 15:15 + Ls] = sq
                for ci in range(Ls // 2048):
                    chunks.append(pad[:, ci * 2048:ci * 2048 + 2078])
            nch = len(chunks)
            nc = _prog("dwconf", lambda: build_dwconf(nch))
            wdw = A(conv_w_dw)[j]
            bdw = A(conv_b_dw)[j]
            maps = []
            for c in range(NCORE):
                u = np.stack([np.stack([ck[c * 128:(c + 1) * 128], ck[1024 + c * 128:1024 + (c + 1) * 128]]) for ck in chunks])
                maps.append(dict(u=_c(u), wdw=_c(np.concatenate([wdw[:, c * 128:(c + 1) * 128].T, bdw[c * 128:(c + 1) * 128, None]], axis=1))))
            res = _run(nc, maps)
            y = np.concatenate([res[c]["y"] for c in range(NCORE)], axis=1)
            fp = np.concatenate([y[ci] for ci in range(LP_ // 2048)], axis=1)
            fs = y[LP_ // 2048:]
            w_h, b_h, pre_ln = A(conv_w_pw2)[j], A(conv_b_pw2)[j], True
            cgb = _c(np.stack([A(conv_ln_g)[j], A(conv_ln_b)[j]]))
        else:
            nc = _prog("lin3072", lambda: build_lin(TPC, 3072))
            maps = [dict(x=xt, w=_c(A(hy_w_in)[j]), b=_c(A(hy_b_in)[j]), identd=ident) for xt in shard_tok(xp, xs)]
            up, us = unshard_feat([r["yT"] for r in _run(nc, maps)])
            U = np.concatenate([up] + [us[s] for s in range(NSAMP)], axis=1)
            nc = _prog("hyena", lambda: build_hyena2(LP_, LS_, NSAMP, 8))
            ztp, decp = hyena_pos(LP_)
            zts, decs = hyena_pos(LS_)
            wsh, bsh = A(hy_w_short)[j], A(hy_b_short)[j]
            fwo = A(hy_f_wout)[j]
            fvec = _c(np.stack([A(hy_f_freq)[j], A(hy_f_b1)[j], A(hy_f_b2)[j], A(hy_f_b3)[j]], axis=1))
            maps = []
            for c in range(NCORE):
                sl = [slice(k * 1024 + c * 128, k * 1024 + (c + 1) * 128) for k in range(3)]
                maps.append(dict(
                    u=_c(np.stack([U[s] for s in sl])),
                    wsh=_c(np.concatenate([wsh[:, s].T for s in sl] + [bsh[s][:, None] for s in sl], axis=1)),
                    fbias=_c(A(hy_f_bias)[j][:, c * 128:(c + 1) * 128].T),
                    zt_p=ztp, zt_s=zts, fw1=_c(A(hy_f_w1)[j]), fw2=_c(A(hy_f_w2)[j]), fw3=_c(A(hy_f_w3)[j]), fvec=fvec,
                    fwout=_c(np.concatenate([fwo[:, nd * 1024 + c * 128:nd * 1024 + (c + 1) * 128] for nd in range(4)], axis=1)),
                    dec_p=_c(decp[:, c * 128:(c + 1) * 128].T), dec_s=_c(decs[:, c * 128:(c + 1) * 128].T),
                    identd=ident, antid=np.ascontiguousarray(ident[::-1])))
            res = _run(nc, maps)
            Z = np.concatenate([res[c]["z"] for c in range(NCORE)], axis=0)
            fp = Z[:, :LP_]
            fs = np.stack([Z[:, LP_ + s * LS_:LP_ + (s + 1) * LS_] for s in range(NSAMP)])
            w_h, b_h, pre_ln, cgb = A(hy_w_out)[j], A(hy_b_out)[j], False, np.zeros((2, D), np.float32)
        nc = _prog("proj%d" % pre_ln, lambda: build_proj(TPC, pre_ln))
        maps = [dict(hT=ht, w=_c(w_h), b=_c(b_h), cgb=cgb) for ht in shard_feat(fp, fs)]
        hsh = [r["h"] for r in _run(nc, maps)]
        nc = _prog("tok", lambda: build_tok(TPC))
        common = tok_weights(A(ln1_g)[i], A(ln1_b)[i], A(ln2_g)[i], A(ln2_b)[i], A(moe_w_group)[i], A(moe_b_group)[i],
                             A(moe_w_expert)[i], A(moe_b_expert)[i], A(moe_w_gate)[i], A(moe_w_up)[i], A(moe_w_down)[i],
                             A(ple_w_up)[i], A(ple_w_gate)[i])
        xsh = shard_tok(xp, xs)
        psh = shard_tok(p_prompt[i, 0], p_sample[i])
        maps = [dict(common, x=xsh[c], h=_c(hsh[c]), p=psh[c]) for c in range(NCORE)]
        xp, xs = unshard_tok([r["xo"] for r in _run(nc, maps)])
    return (np.ascontiguousarray(xp[None].astype(np.float32)), np.ascontiguousarray(xs.astype(np.float32)))


def build_hyena2(LP, LS, NSEQ, GS):
    nc = bass.Bass("TRN2", target_bir_lowering=False)
    S = Sched(nc)
    C = Ctx(nc, S)
    TT = LP + NSEQ * LS
    u_d = C.din("u", [3, 128, TT])
    wsh_d = C.din("wsh", [128, 12])
    fb_d = C.din("fbias", [128, 2])
    ztp_d = C.din("zt_p", [33, LP])
    zts_d = C.din("zt_s", [33, LS])
    w1_d = C.din("fw1", [33, 64])
    w2_d = C.din("fw2", [64, 64])
    w3_d = C.din("fw3", [64, 64])
    fv_d = C.din("fvec", [64, 4])
    wo_d = C.din("fwout", [64, 4 * 128])
    dcp_d = C.din("dec_p", [128, LP])
    dcs_d = C.din("dec_s", [128, LS])
    id_d = C.din("identd", [128, 128])
    aj_d = C.din("antid", [128, 128])
    z_d = C.dout("z", [128, TT])
    uc_d = nc.dram_tensor("uc", [3, 128, TT], F32, kind="Internal")
    gxp_d = nc.dram_tensor("gx_p", [2 * 128, 2 * LP], BF16, kind="Internal")
    gxs_d = nc.dram_tensor("gx_s", [2 * 128, 2 * LS], BF16, kind="Internal")

    def cload(name, shape, src):
        b = S.sbuf(name + "_sb", shape, F32)
        S.dma("sp", b.t[:], src, writes=[b])
        return b
    wsh = cload("wsh", [128, 12], wsh_d.ap())
    fbs = cload("fbias", [128, 2], fb_d.ap())
    w1 = cload("fw1", [33, 64], w1_d.ap())
    w2 = cload("fw2", [64, 64], w2_d.ap())
    w3 = cload("fw3", [64, 64], w3_d.ap())
    fv = cload("fvec", [64, 4], fv_d.ap())
    wo = cload("fwout", [64, 512], wo_d.ap())
    ident = cload("ident", [128, 128], id_d.ap())
    antid = cload("antid", [128, 128], aj_d.ap())
    identb = S.sbuf("identb", [128, 128], BF16)
    antib = S.sbuf("antib", [128, 128], BF16)
    S.op("dve", lambda e: e.tensor_copy(identb.t[:], ident.t[:]), reads=[ident], writes=[identb])
    S.op("dve", lambda e: e.tensor_copy(antib.t[:], antid.t[:]), reads=[antid], writes=[antib])
    fsc = S.sbuf("fsc", [64, 4], F32)
    S.op("dve", lambda e: e.tensor_scalar(fsc.t[:, 0:1], fv.t[:, 0:1], 1.0 / 3.0, None, ALU.mult), reads=[fv], writes=[fsc])
    S.op("dve", lambda e: e.tensor_scalar(fsc.t[:, 1:4], fv.t[:, 1:4], fsc.t[:, 0:1], None, ALU.mult), reads=[fv, fsc], writes=[fsc])

    gt = S.sbuf("gt", [128, 2048], F32)
    sb = S.sbuf("scb", [128, 2050], F32)
    seqs = [(0, LP)] + [(LP + i * LS, LS) for i in range(NSEQ)]
    for sl in range(3):
        for (s0, L) in seqs:
            for ci in range(L // 2048):
                b, o = sb, gt
                t0 = s0 + ci * 2048
                lo = 0 if ci == 0 else -1
                hi = 2048 if ci == L // 2048 - 1 else 2049
                if lo == 0:
                    S.op("pool", lambda e, b=b: e.memset(b.t[:, 0:1], 0.0), writes=[b])
                if hi == 2048:
                    S.op("pool", lambda e, b=b: e.memset(b.t[:, 2049:2050], 0.0), writes=[b])
                S.dma("sp", b.t[:, 1 + lo:1 + hi], u_d.ap()[sl, :, t0 + lo:t0 + hi], reads=[b], writes=[b])
                S.op("dve", lambda e, b=b, o=o, sl=sl: e.tensor_scalar(o.t[:], b.t[:, 0:2048], wsh.t[:, 3 * sl:3 * sl + 1], wsh.t[:, 9 + sl:10 + sl], ALU.mult, ALU.add),
                     reads=[b, wsh], writes=[o])
                for j in (1, 2):
                    S.op("dve", lambda e, b=b, o=o, sl=sl, j=j: e.scalar_tensor_tensor(o.t[:], b.t[:, j:j + 2048], wsh.t[:, 3 * sl + j:3 * sl + j + 1], o.t[:], ALU.mult, ALU.add),
                         reads=[b, wsh, o], writes=[o])
                S.dma("sp", uc_d.ap()[sl, :, t0:t0 + 2048], o.t[:], reads=[o], writes=[S.dbuf("uc%d_%d" % (sl, t0))])

    zt = [S.sbuf("zt%d" % i, [33, 512], F32) for i in range(2)]
    hm = [S.sbuf("hm%d" % i, [64, 512], F32) for i in range(3)]
    tq = S.sbuf("tq", [64, 512], F32)
    dc = [S.sbuf("dc%d" % i, [128, 512], F32) for i in range(2)]
    f1 = S.sbuf("f1", [128, 512], F32)
    f0 = S.sbuf("f0", [128, 512], F32)
    ftb = S.sbuf("ftb", [128, 4, 128], F32)
    rb = [S.sbuf("rb%d" % i, [128, 512], BF16) for i in range(2)]
    hb0 = S.sbuf("hb0", [128, 2], F32)
    pst = S.psum("pst", [128, 512], F32)
    psm = [S.psum("psm%d" % i, [128, 512], F32) for i in range(2)]
    psf = S.psum("psf", [128, 512], F32)
    prv = S.psum("prv", [128, 512], F32)
    pacc = [S.psum("pacc%d" % i, [128, 512], F32) for i in range(2)]
    pyt = S.psum("pyt", [128, 512], BF16)
    k = 0
    for (L, zsrc, dsrc, gdst, tag) in ((LP, ztp_d, dcp_d, gxp_d, "p"), (LS, zts_d, dcs_d, gxs_d, "s")):
        gxb = S.dbuf("gx" + tag)
        for blk in range(L // 512):
            z, d = zt[blk % 2], dc[blk % 2]
            S.dma("sp", z.t[:], zsrc.ap()[:, blk * 512:(blk + 1) * 512], writes=[z])
            S.dma("sp", d.t[:], dsrc.ap()[:, blk * 512:(blk + 1) * 512], writes=[d])
            src, srcb, K_ = z.t[:], z, 33
            for li, wl in enumerate((w1, w2, w3)):
                pm, h = psm[li % 2], hm[li]
                S.op("pe", lambda e, pm=pm, wl=wl, src=src, K_=K_: e.matmul(pm.t[0:64, :], wl.t[0:K_, :], src, start=True, stop=True), reads=[wl, srcb], writes=[pm])
                S.op("act", lambda e, pm=pm, h=h, li=li: e.activation(h.t[:], pm.t[0:64, :], AF.Sin, bias=fsc.t[:, 1 + li:2 + li], scale=fsc.t[:, 0:1]), reads=[pm, fsc], writes=[h])
                S.op("dve", lambda e, h=h: e.tensor_tensor(tq.t[:], h.t[:], h.t[:], ALU.mult), reads=[h], writes=[tq])
                S.op("dve", lambda e: e.tensor_scalar(tq.t[:], tq.t[:], -4.0, 3.0, ALU.mult, ALU.add), reads=[tq], writes=[tq])
                S.op("dve", lambda e, h=h: e.tensor_tensor(h.t[:], h.t[:], tq.t[:], ALU.mult), reads=[h, tq], writes=[h])
                src, srcb, K_ = h.t[:], h, 64
            for n in range(2):
                S.op("pe", lambda e, n=n: e.matmul(psf.t[:], wo.t[:, (2 * n + 1) * 128:(2 * n + 2) * 128], hm[2].t[:], start=True, stop=True), reads=[wo, hm[2]], writes=[psf])
                S.op("dve", lambda e, d=d: e.tensor_tensor(f1.t[:], psf.t[:], d.t[:], ALU.mult), reads=[psf, d], writes=[f1])
                if blk == 0:
                    S.op("dve", lambda e, n=n: e.tensor_copy(hb0.t[:, n:n + 1], f1.t[:, 0:1]), reads=[f1], writes=[hb0])
                for kk in range(4):
                    S.op("pe", lambda e, kk=kk: e.transpose(pst.t[:, kk * 128:(kk + 1) * 128], f1.t[:, kk * 128:(kk + 1) * 128], ident.t[:]), reads=[f1, ident], writes=[pst])
                S.op("act", lambda e: e.activation(ftb.t[:], pst.t[:].rearrange("p (a b) -> p a b", a=4), AF.Copy), reads=[pst], writes=[ftb])
                for kk in range(4):
                    S.op("pe", lambda e, kk=kk: e.matmul(prv.t[:, (3 - kk) * 128:(4 - kk) * 128], ftb.t[:, kk, :], antid.t[:], start=True, stop=True), reads=[ftb, antid], writes=[prv])
                r = rb[k % 2]
                k += 1
                S.op("act", lambda e, r=r: e.activation(r.t[:], prv.t[:], AF.Copy), reads=[prv], writes=[r])
                u0 = L - 512 - blk * 512
                ncol = 511 if blk == 0 else 512
                S.dma("sp", gdst.ap()[n * 128:(n + 1) * 128, u0:u0 + ncol], r.t[:, 0:ncol], reads=[r], writes=[gxb])
                S.op("pe", lambda e, n=n: e.matmul(psf.t[:], wo.t[:, (2 * n) * 128:(2 * n + 1) * 128], hm[2].t[:], start=True, stop=True), reads=[wo, hm[2]], writes=[psf])
                S.op("dve", lambda e, d=d: e.tensor_tensor(f0.t[:], psf.t[:], d.t[:], ALU.mult), reads=[psf, d], writes=[f0])
                if blk == 0:
                    S.op("dve", lambda e, n=n: e.tensor_tensor(f0.t[:, 0:1], f0.t[:, 0:1], hb0.t[:, n:n + 1], ALU.add), reads=[f0, hb0], writes=[f0])
                r = rb[k % 2]
                k += 1
                S.op("act", lambda e, r=r: e.activation(r.t[:], f0.t[:], AF.Copy), reads=[f0], writes=[r])
                S.dma("sp", gdst.ap()[n * 128:(n + 1) * 128, L - 1 + blk * 512:L - 1 + (blk + 1) * 512], r.t[:], reads=[r], writes=[gxb])

    NBLK = max(LP, GS * LS) // 128
    vbuf = S.sbuf("vbuf", [128, NBLK * 128], F32)
    W = S.sbuf("W", [128, NBLK, 128], BF16)
    ytok = S.sbuf("ytok", [128, NBLK, 128], BF16)
    NDC = 32
    strip = [S.sbuf("strip%d" % i, [128, NDC * 128], BF16) for i in range(2)]
    wn = S.sbuf("wn", [128, 512], BF16)
    tmp = S.sbuf("tmpg", [128, 512], F32)
    groups = [(0, LP, 1, gxp_d, "p")] + [(LP + gi * GS * LS, LS, GS, gxs_d, "s") for gi in range(NSEQ // GS)]
    kk_ = 0
    for (s0, L, ns, gsrc, tag) in groups:
        nb = L // 128
        nblk = ns * nb
        mi0 = nb - 1
        nd = 2 * nb - 1
        chunks = [(c0, min(c0 + NDC, nd)) for c0 in range(0, nd, NDC)]
        chunks.sort(key=lambda cc: 0 if cc[0] <= mi0 < cc[1] else 1)
        WV = W.t[:, 0:nblk, :].rearrange("p (s b) c -> p s b c", s=ns)
        S.dma("sp", vbuf.t[:, 0:ns * L], uc_d.ap()[2, :, s0:s0 + ns * L],
              reads=[S.dbuf("uc2_%d" % t) for t in range(s0, s0 + ns * L, 2048)], writes=[vbuf])
        for n in range(2):
            for q in range(nblk // 4):
                for j in range(4):
                    S.op("pe", lambda e, q=q, j=j: e.transpose(pst.t[:, j * 128:(j + 1) * 128], vbuf.t[:, (4 * q + j) * 128:(4 * q + j + 1) * 128], ident.t[:]),
                         reads=[vbuf, ident], writes=[pst])
                S.op("act", lambda e: e.activation(wn.t[:], pst.t[:], AF.Copy), reads=[pst], writes=[wn])
                S.op("pe", lambda e: e.matmul(prv.t[:], antib.t[:], wn.t[:], start=True, stop=True), reads=[antib, wn], writes=[prv])
                S.op("dve", lambda e, q=q: e.tensor_copy(W.t[:, 4 * q:4 * q + 4, :], prv.t[:].rearrange("p (a b) -> p a b", a=4)), reads=[prv], writes=[W])
            for ch in range(128):
                pa = pacc[ch % 2]
                PV = pa.t[:, 0:nblk].rearrange("p (s a) -> p s a", s=ns)
                first = True
                nmm = 0
                for (c0, c1) in chunks:
                    st = strip[kk_ % 2]
                    kk_ += 1
                    S.dma("sp", st.t[:, 0:(c1 - c0) * 128], bass.AP(gsrc, (n * 128 + ch) * 2 * L + c0 * 128, [[1, 128], [1, (c1 - c0) * 128]]),
                          reads=[S.dbuf("gx" + tag)], writes=[st])
                    mis = list(range(c0, c1))
                    if c0 <= mi0 < c1:
                        mis.remove(mi0)
                        mis.insert(0, mi0)
                    for mi in mis:
                        m = mi - mi0
                        b0, b1 = max(0, -m), min(nb, nb - m)
                        nmm += 1
                        S.op("pe", lambda e, PV=PV, st=st, mi=mi, c0=c0, b0=b0, b1=b1, m=m, ch=ch, first=first, last=(nmm == nd), WV=WV:
                             e.matmul(PV[:, :, b0 + m:b1 + m], st.t[:, (mi - c0) * 128:(mi - c0 + 1) * 128], WV[:, :, b0:b1, ch], start=first, stop=last),
                             reads=[st, W], writes=[pa])
                        first = False
                S.op("act", lambda e, pa=pa, ch=ch, nblk=nblk: e.activation(ytok.t[:, 0:nblk, ch], pa.t[:, 0:nblk], AF.Copy), reads=[pa], writes=[ytok])
            for q in range(nblk // 4):
                if q % 4 == 0:
                    tok = s0 + q * 512
                    S.dma("sp", gt.t[:], uc_d.ap()[n, :, tok:tok + 2048], reads=[S.dbuf("uc%d_%d" % (n, tok))], writes=[gt])
                for j in range(4):
                    S.op("pe", lambda e, q=q, j=j: e.transpose(pyt.t[:, j * 128:(j + 1) * 128], ytok.t[:, 4 * q + j, :], identb.t[:]), reads=[ytok, identb], writes=[pyt])
                vs = vbuf.t[:, q * 512:(q + 1) * 512]
                S.op("dve", lambda e, vs=vs, n=n: e.scalar_tensor_tensor(tmp.t[:], vs, fbs.t[:, n:n + 1], pyt.t[:], ALU.mult, ALU.add), reads=[vbuf, fbs, pyt], writes=[tmp])
                S.op("dve", lambda e, vs=vs, q=q: e.tensor_tensor(vs, tmp.t[:], gt.t[:, (q % 4) * 512:(q % 4 + 1) * 512], ALU.mult), reads=[tmp, gt], writes=[vbuf])
        S.dma("sp", z_d.ap()[:, s0:s0 + ns * L], vbuf.t[:, 0:ns * L], reads=[vbuf], writes=[S.dbuf("z%d" % s0)], final=True)
    S.emit()
    return nc
```
